# Optimizing a Trainium2 kernel written in Bass

```python
import math
import jax
import jax.numpy as jnp
from jax import lax
import numpy as np

D_MODEL = 1024
BATCH = 32
SEQ = 2048
DEPTH = 4

CHUNK = 64
Q_BLOCK = 128
HEAD_DIM = 64
N_HEADS = D_MODEL // HEAD_DIM
KV_DIM = 64
IDX_HEADS = 8
IDX_DIM = 64
INDEX_TOPK = 256
RNN_WIDTH = D_MODEL
RNN_BLOCKS = 8
RNN_BW = RNN_WIDTH // RNN_BLOCKS
CONV_W = 4
LRU_C = 8.0
REL_BUCKETS = 32
REL_MAX_DIST = 128
D_FF = 2816
N_EXPERTS = 8
TOP_K = 2
D_FF_EXPERT = 3584
PLE_DIM = 256
LN_EPS = 1e-5
DN_ALPHA = (2 * DEPTH) ** 0.25
DN_BETA = (8 * DEPTH) ** -0.25
SPLITS = (N_HEADS * HEAD_DIM, KV_DIM, KV_DIM, IDX_HEADS * IDX_DIM, IDX_DIM, IDX_HEADS, RNN_WIDTH, RNN_WIDTH, D_MODEL, D_MODEL)
D_IN = sum(SPLITS)
SPLIT_OFFSETS = tuple(int(o) for o in np.cumsum(SPLITS)[:-1])
N_DENSE = (DEPTH + 1) // 2
N_MOE = DEPTH // 2

kernel_name = 'hybrid_dsa_rglru_moe_deepnorm'


def layer_norm(x, g, b):
    xf = x.astype(jnp.float32)
    mu = jnp.mean(xf, axis=-1, keepdims=True)
    var = jnp.mean(jnp.square(xf - mu), axis=-1, keepdims=True)
    return ((xf - mu) * lax.rsqrt(var + LN_EPS)).astype(x.dtype) * g + b


def t5_bucket(rel):
    half = REL_BUCKETS // 2
    max_exact = half // 2
    ret = jnp.where(rel > 0, half, 0)
    n = jnp.abs(rel)
    nf = jnp.maximum(n, 1).astype(jnp.float32)
    large = max_exact + (jnp.log(nf / max_exact) / math.log(REL_MAX_DIST / max_exact) * (half - max_exact)).astype(jnp.int32)
    large = jnp.minimum(large, half - 1)
    return ret + jnp.where(n < max_exact, n, large)


def dsa_attention(q, k, v, q_idx, k_idx, w_idx, rel_bias):
    b, s, h, dh = q.shape
    k_top = min(INDEX_TOPK, s // 4)
    nb = s // Q_BLOCK
    key_chunk = jnp.arange(s) // CHUNK

    def to_blocks(t):
        return jnp.moveaxis(t.reshape(b, nb, Q_BLOCK, *t.shape[2:]), 1, 0)

    def block(args):
        qb, qib, wb, start = args
        qpos = start + jnp.arange(Q_BLOCK)
        qchunk = qpos // CHUNK
        admiss = key_chunk[None, :] <= qchunk[:, None]
        dots = jnp.einsum('bqhd,bsd->bqhs', qib, k_idx).astype(jnp.float32) * (IDX_DIM ** -0.5)
        score = jnp.einsum('bqhs,bqh->bqs', jax.nn.relu(dots), wb.astype(jnp.float32)) * (IDX_HEADS ** -0.5)
        score = jnp.where(admiss[None], score, -jnp.inf)
        _, idx = lax.top_k(score, k_top)
        kg = jax.vmap(lambda kk, ii: kk[ii])(k, idx)
        vg = jax.vmap(lambda vv, ii: vv[ii])(v, idx)
        logits = jnp.einsum('bqhd,bqkd->bqhk', qb, kg).astype(jnp.float32) * (dh ** -0.5)
        bias = rel_bias[t5_bucket(idx - qpos[None, :, None])]
        logits = logits + jnp.moveaxis(bias, -1, 2).astype(jnp.float32)
        valid = (idx // CHUNK) <= qchunk[None, :, None]
        logits = jnp.where(valid[:, :, None, :], logits, -1e30)
        probs = jax.nn.softmax(logits, axis=-1).astype(v.dtype)
        return jnp.einsum('bqhk,bqkd->bqhd', probs, vg)

    starts = jnp.arange(nb, dtype=jnp.int32) * Q_BLOCK
    out = lax.map(block, (to_blocks(q), to_blocks(q_idx), to_blocks(w_idx), starts))
    return jnp.moveaxis(out, 0, 1).reshape(b, s, h * dh)


def causal_conv(x, w, bias):
    s = x.shape[1]
    xp = jnp.pad(x, ((0, 0), (CONV_W - 1, 0), (0, 0)))
    out = xp[:, 0:s] * w[0]
    for j in range(1, CONV_W):
        out = out + xp[:, j:j + s] * w[j]
    return out + bias


def rg_lru(xc, wa, ba, wx, bx, lam):
    b, s, d = xc.shape
    xb = xc.reshape(b, s, RNN_BLOCKS, RNN_BW)
    r = jax.nn.sigmoid(jnp.einsum('bsnc,ncd->bsnd', xb, wa).reshape(b, s, d) + ba)
    i = jax.nn.sigmoid(jnp.einsum('bsnc,ncd->bsnd', xb, wx).reshape(b, s, d) + bx)
    log_a = -LRU_C * r.astype(jnp.float32) * jax.nn.softplus(-lam.astype(jnp.float32))
    a = jnp.exp(log_a)
    u = jnp.sqrt(-jnp.expm1(2.0 * log_a)) * (i * xc).astype(jnp.float32)

    def combine(c1, c2):
        a1, b1 = c1
        a2, b2 = c2
        return a1 * a2, a2 * b1 + b2

    _, hs = lax.associative_scan(combine, (a, u), axis=1)
    return hs.astype(xc.dtype)


def token_mixer(x, w_in, conv_w, conv_b, lru_wa, lru_ba, lru_wx, lru_bx, lru_lam, w_br_attn, w_br_rnn, w_o, rel_bias):
    b, s, _ = x.shape
    z = x @ w_in
    q, k, v, qi, ki, wi, xr, yr, ga, gr = jnp.split(z, SPLIT_OFFSETS, axis=-1)
    q = q.reshape(b, s, N_HEADS, HEAD_DIM)
    qi = qi.reshape(b, s, IDX_HEADS, IDX_DIM)
    o_attn = dsa_attention(q, k, v, qi, ki, wi, rel_bias)
    xc = causal_conv(xr, conv_w, conv_b)
    o_rnn = rg_lru(xc, lru_wa, lru_ba, lru_wx, lru_bx, lru_lam) * jax.nn.gelu(yr)
    merged = jax.nn.sigmoid(ga) * (o_attn @ w_br_attn) + jax.nn.sigmoid(gr) * (o_rnn @ w_br_rnn)
    return merged @ w_o


def swiglu(x, w_gate, w_up, w_down):
    return (jax.nn.silu(x @ w_gate) * (x @ w_up)) @ w_down


def moe_swiglu(x, router, router_b, w_gate, w_up, w_down):
    logits = (x @ router).astype(jnp.float32) + router_b.astype(jnp.float32)
    top_v, top_i = lax.top_k(logits, TOP_K)
    gates = jax.nn.softmax(top_v, axis=-1)
    combine = jnp.sum(jax.nn.one_hot(top_i, N_EXPERTS, dtype=jnp.float32) * gates[..., None], axis=-2).astype(x.dtype)
    out = jnp.zeros_like(x)
    for e in range(N_EXPERTS):
        out = out + combine[..., e:e + 1] * swiglu(x, w_gate[e], w_up[e], w_down[e])
    return out


def setup_inputs(seed: int = 0) -> dict:
    key = jax.random.key(seed)
    ks = jax.random.split(key, 28)
    f32 = jnp.float32

    def nrm(k, shape, scale):
        return jax.random.normal(k, shape, f32) * scale

    a0 = jax.random.uniform(ks[9], (DEPTH, RNN_WIDTH), f32, 0.9, 0.999)
    return {
        'x': nrm(ks[0], (BATCH, SEQ, D_MODEL), 1.0),
        'p': nrm(ks[1], (DEPTH, BATCH, SEQ, PLE_DIM), 1.0),
        'w_in': nrm(ks[2], (DEPTH, D_MODEL, D_IN), D_MODEL ** -0.5),
        'conv_w': nrm(ks[3], (DEPTH, CONV_W, RNN_WIDTH), CONV_W ** -0.5),
        'conv_b': nrm(ks[4], (DEPTH, RNN_WIDTH), 0.01),
        'lru_wa': nrm(ks[5], (DEPTH, RNN_BLOCKS, RNN_BW, RNN_BW), RNN_BW ** -0.5),
        'lru_ba': nrm(ks[6], (DEPTH, RNN_WIDTH), 0.01),
        'lru_wx': nrm(ks[7], (DEPTH, RNN_BLOCKS, RNN_BW, RNN_BW), RNN_BW ** -0.5),
        'lru_bx': nrm(ks[8], (DEPTH, RNN_WIDTH), 0.01),
        'lru_lam': jnp.log(a0) - jnp.log1p(-a0),
        'w_br_attn': nrm(ks[10], (DEPTH, N_HEADS * HEAD_DIM, D_MODEL), (N_HEADS * HEAD_DIM) ** -0.5),
        'w_br_rnn': nrm(ks[11], (DEPTH, RNN_WIDTH, D_MODEL), RNN_WIDTH ** -0.5),
        'w_o': nrm(ks[12], (DEPTH, D_MODEL, D_MODEL), DN_BETA * D_MODEL ** -0.5),
        'rel_bias': nrm(ks[13], (REL_BUCKETS, N_HEADS), 0.5),
        'ln1_g': 1.0 + nrm(ks[14], (DEPTH, D_MODEL), 0.02),
        'ln1_b': nrm(ks[15], (DEPTH, D_MODEL), 0.01),
        'ffn_w_gate': nrm(ks[16], (N_DENSE, D_MODEL, D_FF), D_MODEL ** -0.5),
        'ffn_w_up': nrm(ks[17], (N_DENSE, D_MODEL, D_FF), D_MODEL ** -0.5),
        'ffn_w_down': nrm(ks[18], (N_DENSE, D_FF, D_MODEL), DN_BETA * D_FF ** -0.5),
        'moe_router': nrm(ks[19], (N_MOE, D_MODEL, N_EXPERTS), D_MODEL ** -0.5),
        'moe_router_b': nrm(ks[20], (N_MOE, N_EXPERTS), 0.01),
        'moe_w_gate': nrm(ks[21], (N_MOE, N_EXPERTS, D_MODEL, D_FF_EXPERT), D_MODEL ** -0.5),
        'moe_w_up': nrm(ks[22], (N_MOE, N_EXPERTS, D_MODEL, D_FF_EXPERT), D_MODEL ** -0.5),
        'moe_w_down': nrm(ks[23], (N_MOE, N_EXPERTS, D_FF_EXPERT, D_MODEL), DN_BETA * D_FF_EXPERT ** -0.5),
        'ple_w_gate': nrm(ks[24], (DEPTH, D_MODEL, D_MODEL), D_MODEL ** -0.5),
        'ple_w_proj': nrm(ks[25], (DEPTH, PLE_DIM, D_MODEL), DN_BETA * PLE_DIM ** -0.5),
        'ln2_g': 1.0 + nrm(ks[26], (DEPTH, D_MODEL), 0.02),
        'ln2_b': nrm(ks[27], (DEPTH, D_MODEL), 0.01),
    }


def reference(x, p, w_in, conv_w, conv_b, lru_wa, lru_ba, lru_wx, lru_bx, lru_lam, w_br_attn, w_br_rnn, w_o, rel_bias, ln1_g, ln1_b, ffn_w_gate, ffn_w_up, ffn_w_down, moe_router, moe_router_b, moe_w_gate, moe_w_up, moe_w_down, ple_w_gate, ple_w_proj, ln2_g, ln2_b):
    for i in range(DEPTH):
        mix = token_mixer(x, w_in[i], conv_w[i], conv_b[i], lru_wa[i], lru_ba[i], lru_wx[i], lru_bx[i], lru_lam[i], w_br_attn[i], w_br_rnn[i], w_o[i], rel_bias)
        x = layer_norm(DN_ALPHA * x + mix, ln1_g[i], ln1_b[i])
        j = i // 2
        if i % 2 == 0:
            f = swiglu(x, ffn_w_gate[j], ffn_w_up[j], ffn_w_down[j])
        else:
            f = moe_swiglu(x, moe_router[j], moe_router_b[j], moe_w_gate[j], moe_w_up[j], moe_w_down[j])
        ple = jax.nn.sigmoid(x @ ple_w_gate[i]) * (p[i] @ ple_w_proj[i])
        x = layer_norm(DN_ALPHA * x + f + ple, ln2_g[i], ln2_b[i])
    return x
```

```python
import numpy as np
from contextlib import ExitStack
import concourse.bass as bass
import concourse.mybir as mybir
from concourse.bass_utils import run_bass_kernel_spmd

F32 = mybir.dt.float32
BF16 = mybir.dt.bfloat16
AF = mybir.ActivationFunctionType
ALU = mybir.AluOpType

S = 2048
D = 1024
NB = 16
NKC = 8
DEPTH = 4
D_FF = 2816
D_FFE = 3584
NEXP = 8
PLE = 256
DIN = 5832
OFF = dict(q=0, k=1024, v=1088, qi=1152, ki=1664, wi=1728, xr=1736, yr=2760, ga=3784, gr=4808)
ALPHA = float((2 * DEPTH) ** 0.25)
EPS = 1e-5
NEG = -1.0e30
MASKMAG = 30000.0
NBIS = 22
N_CORES = 8


class Trk:
    def __init__(self, nc, stack):
        self.nc = nc
        self.stack = stack
        self.engs = {'pe': nc.tensor, 'act': nc.scalar, 'dve': nc.vector, 'pool': nc.gpsimd, 'sp': nc.sync}
        self.esem = {}
        self.ecnt = {}
        self.nsem = 0
        for e in self.engs:
            self.esem[e] = self._newsem(e)
            self.ecnt[e] = 0
        self.waited = {}
        self.lastw = {}
        self.readers = {}
        self.dsem = {}
        self.dcnt = {}
        self.n_ins = 0
        self.dead = False

    def _newsem(self, tag):
        self.nsem += 1
        return self.stack.enter_context(self.nc.semaphore("s%d_%s" % (self.nsem, tag)))

    def _wait(self, eng, ev):
        sem, val, src = ev
        if src.startswith('dma:') and self.dsem.get(src[4:]) is sem:
            val = max(val, self.dcnt[src[4:]])
        k = (eng, sem.name if hasattr(sem, 'name') else id(sem))
        if self.waited.get(k, 0) >= val:
            return
        self.engs[eng].wait_ge(sem, val)
        self.waited[k] = val

    def _deps(self, eng, reads, writes):
        deps = []
        for r in reads:
            ev = self.lastw.get(r)
            if ev is not None:
                deps.append(ev)
        for w in writes:
            ev = self.lastw.get(w)
            if ev is not None:
                deps.append(ev)
            for ev in self.readers.get(w, {}).values():
                deps.append(ev)
        for ev in deps:
            if ev[2] == eng:
                if eng == 'pe':
                    continue
                if ev[0] is self.esem[eng] and self.ecnt[eng] - ev[1] >= 3:
                    continue
            self._wait(eng, ev)

    def _record(self, ev, key, reads, writes):
        for r in reads:
            self.readers.setdefault(r, {})[key] = ev
        for w in writes:
            self.lastw[w] = ev
            self.readers[w] = {}

    def op(self, eng, fn, reads=(), writes=()):
        if self.dead:
            return None
        self._deps(eng, reads, writes)
        ins = fn(self.engs[eng])
        if self.ecnt[eng] >= 60000:
            self.esem[eng] = self._newsem(eng)
            self.ecnt[eng] = 0
        self.ecnt[eng] += 1
        ins.then_inc(self.esem[eng], 1)
        self.n_ins += 1
        ev = (self.esem[eng], self.ecnt[eng], eng)
        self._record(ev, eng, reads, writes)
        return ev

    def dma(self, q, out, in_, key, reads=(), writes=(), slow=False):
        if self.dead:
            return None
        self._deps(q, reads, writes)
        if key not in self.dsem:
            self.dsem[key] = self._newsem("d")
            self.dcnt[key] = 0
        if self.dcnt[key] >= 60000 * 16:
            self.dsem[key] = self._newsem("d")
            self.dcnt[key] = 0
        if self.dcnt[key] > 0:
            self._wait(q, (self.dsem[key], self.dcnt[key], 'dma:' + key))
        if slow:
            ins = self.engs[q].dma_start(out=out, in_=in_, allow_slow_non_contiguous=True)
        else:
            ins = self.engs[q].dma_start(out=out, in_=in_)
        self.dcnt[key] += 16
        ins.then_inc(self.dsem[key], 16)
        self.n_ins += 1
        ev = (self.dsem[key], self.dcnt[key], 'dma:' + key)
        self._record(ev, 'dma:' + key, reads, writes)
        return ev

    def barrier(self, engines=None):
        if self.dead:
            return
        evs = []
        for e in self.engs:
            if self.ecnt[e] > 0:
                evs.append((self.esem[e], self.ecnt[e], e))
        for k in self.dsem:
            if self.dcnt[k] > 0:
                evs.append((self.dsem[k], self.dcnt[k], 'dma:' + k))
        for e in self.engs:
            for ev in evs:
                if ev[2] == e:
                    continue
                self._wait(e, ev)
        self.lastw = {}
        self.readers = {}

    def final_wait(self, eng='sp'):
        if self.dead:
            return
        for k in self.dsem:
            if self.dcnt[k] > 0:
                self._wait(eng, (self.dsem[k], self.dcnt[k], 'dma:' + k))
        for e in self.engs:
            if e != eng and self.ecnt[e] > 0:
                self._wait(eng, (self.esem[e], self.ecnt[e], e))


def t5_bucket_np(rel):
    half = 16
    max_exact = 8
    ret = np.where(rel > 0, half, 0)
    n = np.abs(rel)
    nf = np.maximum(n, 1).astype(np.float32)
    large = max_exact + (np.log(nf / np.float32(max_exact)) / np.float32(np.log(128 / max_exact)) * (half - max_exact)).astype(np.int32)
    large = np.minimum(large, half - 1)
    return ret + np.where(n < max_exact, n, large)


def head_of(g, i):
    return 2 * ((g // 2) * 4 + i) + (g % 2)


def make_consts():
    c = {}
    c['ident'] = np.eye(128, dtype=np.float32)
    c['i4'] = np.tile(np.eye(128, dtype=np.float32) * MASKMAG, (1, 4))
    q = np.arange(128)[:, None]
    s = np.arange(128)[None, :]
    adm = (s < 64) | (q >= 64)
    c['admneg'] = np.where(adm, 0.0, NEG).astype(np.float32)
    c['adm01'] = np.where(adm, 0.0, -1.0).astype(np.float32)
    return c


def bias_index_tables():
    sl = np.arange(128)[:, None]
    ql = np.arange(128)[None, :]
    out = []
    for kind in range(2):
        rel = sl - ql - 128 * kind
        out.append(t5_bucket_np(rel))
    return out


class StopBuild(Exception):
    pass


def build(nseq, layers, dbg=None, stop=None):
    nc = bass.Bass("TRN2", target_bir_lowering=False)
    NT = nseq * S
    dr = {}

    def din(name, shape, dt=F32):
        dr[name] = nc.dram_tensor(name, list(shape), dt, kind="ExternalInput").ap()
        return dr[name]

    x_in = din('x', [NT, D])
    p_in = din('p', [DEPTH, NT, PLE])
    w_in = din('w_in', [DEPTH, D, DIN])
    conv_w = din('conv_w', [DEPTH, 4, D])
    conv_b = din('conv_b', [DEPTH, D])
    lru_wa = din('lru_wa', [DEPTH, 8, 128, 128])
    lru_ba = din('lru_ba', [DEPTH, D])
    lru_wx = din('lru_wx', [DEPTH, 8, 128, 128])
    lru_bx = din('lru_bx', [DEPTH, D])
    lru_lam = din('lru_lam', [DEPTH, D])
    w_br_attn = din('w_br_attn', [DEPTH, D, D])
    w_br_rnn = din('w_br_rnn', [DEPTH, D, D])
    w_o = din('w_o', [DEPTH, D, D])
    ln1_g = din('ln1_g', [DEPTH, D])
    ln1_b = din('ln1_b', [DEPTH, D])
    ffn_w_gate = din('ffn_w_gate', [2, D, D_FF])
    ffn_w_up = din('ffn_w_up', [2, D, D_FF])
    ffn_w_down = din('ffn_w_down', [2, D_FF, D])
    moe_router = din('moe_router', [2, D, NEXP])
    moe_router_b = din('moe_router_b', [2, NEXP])
    moe_w_gate = din('moe_w_gate', [2, NEXP, D, D_FFE])
    moe_w_up = din('moe_w_up', [2, NEXP, D, D_FFE])
    moe_w_down = din('moe_w_down', [2, NEXP, D_FFE, D])
    ple_w_gate = din('ple_w_gate', [DEPTH, D, D])
    ple_w_proj = din('ple_w_proj', [DEPTH, PLE, D])
    ln2_g = din('ln2_g', [DEPTH, D])
    ln2_b = din('ln2_b', [DEPTH, D])
    c_ident = din('c_ident', [128, 128])
    c_i4 = din('c_i4', [128, 512])
    c_admneg = din('c_admneg', [128, 128])
    c_adm01 = din('c_adm01', [128, 128])
    c_biasg = din('c_biasg', [2, 128, 16 * 128])
    c_biasc = din('c_biasc', [128, 16 * 128])
    y_out = nc.dram_tensor('y', [NT, D], F32, kind="ExternalOutput").ap()
    xs_d = nc.dram_tensor('xs', [S, D], F32, kind="Internal").ap()
    x1s_d = nc.dram_tensor('x1s', [S, D], F32, kind="Internal").ap()
    dbg_out = {}
    if dbg:
        for name, shape in dbg.items():
            dbg_out[name] = nc.dram_tensor('dbg_' + name, list(shape), F32, kind="ExternalOutput").ap()

    with ExitStack() as top:
        T = Trk(nc, top)

        sbn = [0]

        def sb(stack, name, shape, dt):
            sbn[0] += 1
            return stack.enter_context(nc.sbuf_tensor("%s_%d" % (name, sbn[0]), list(shape), dt))

        ps = top.enter_context(nc.psum_tensor("ps", [128, 8, 512], F32))
        bank_list = [list(range(8))]
        bank_ctr = [0]

        def next_bank():
            bl = bank_list[0]
            b = bl[bank_ctr[0] % len(bl)]
            bank_ctr[0] += 1
            return b

        evac_ctr = [0]

        def evac_eng():
            evac_ctr[0] += 1
            return 'act' if evac_ctr[0] % 2 == 0 else 'dve'

        def copy_op(eng, out, in_, reads, writes):
            if eng == 'act':
                return T.op('act', lambda e: e.activation(out=out, in_=in_, func=AF.Copy), reads, writes)
            return T.op(eng, lambda e: e.tensor_copy(out, in_), reads, writes)

        IDENT = sb(top, "IDENT", [128, 128], BF16)
        I4 = sb(top, "I4", [128, 512], BF16)
        ADMNEG = sb(top, "ADMNEG", [128, 128], F32)
        ADM01 = sb(top, "ADM01", [128, 128], BF16)
        BIAS = sb(top, "BIAS", [128, 2, 2048], BF16)
        XT = sb(top, "XT", [128, NKC, S], BF16)
        CW = sb(top, "CW", [128, 4, 8], F32)
        CB = sb(top, "CB", [128, 8], F32)
        BA = sb(top, "BA", [128, 8], F32)
        BX = sb(top, "BX", [128, 8], F32)
        LAM = sb(top, "LAM", [128, 8], F32)
        CL = sb(top, "CL", [128, 8], F32)
        CL2 = sb(top, "CL2", [128, 8], F32)
        RB = sb(top, "RB", [128, 8], F32)
        EPSB = sb(top, "EPSB", [128, 1], F32)
        T.op('pool', lambda e: e.memset(EPSB[:], EPS), writes=['EPSB'])

        T.dma('pool', IDENT[:], c_ident, 'c0', writes=['IDENT'])
        T.dma('pool', I4[:], c_i4, 'c0', writes=['I4'])
        T.dma('pool', ADM01[:], c_adm01, 'c0', writes=['ADM01'])
        T.dma('sp', ADMNEG[:], c_admneg, 'c1', writes=['ADMNEG'])
        with ExitStack() as st:
            BG = sb(st, "BG", [128, 2, 2048], F32)
            BC = sb(st, "BC", [128, 2048], F32)
            T.dma('sp', BG[:, 0, :], c_biasg[0], 'c1', writes=['BG'])
            T.dma('sp', BG[:, 1, :], c_biasg[1], 'c1', writes=['BG'])
            T.dma('sp', BC[:], c_biasc, 'c1', writes=['BC'])
            for k in range(2):
                T.op('dve', lambda e, k=k: e.tensor_tensor(out=BG[:, k, :], in0=BG[:, k, :], in1=BC[:], op=ALU.subtract),
                     reads=['BC', 'BG'], writes=['BG'])
                T.op('dve', lambda e, k=k: e.tensor_scalar(out=BIAS[:, k, :], in0=BG[:, k, :], scalar1=8.0, scalar2=None, op0=ALU.mult),
                     reads=['BG'], writes=['BIAS'])
            T.barrier()

        def load_w(dst, src2d, key, wregion):
            T.dma('pool', dst, src2d.rearrange("(kc p) n -> p kc n", p=128), key, writes=[wregion])

        def build_xt(stack_parent, dstT, src_tok, nkc, tag):
            with ExitStack() as st:
                XB = sb(st, "XB" + tag, [128, 2, 4, nkc * 128], BF16)
                for tg in range(4):
                    sl = tg % 2
                    T.dma('pool', XB[:, sl, :, :],
                          src_tok[tg * 512:(tg + 1) * 512, :].rearrange("(t p) f -> p t f", p=128),
                          'xb%d' % sl, writes=[('XB', sl)])
                    for kc in range(nkc):
                        b = next_bank()
                        for t in range(4):
                            T.op('pe', lambda e, b=b, t=t, kc=kc, sl=sl: e.matmul(
                                ps[:, b, t * 128:(t + 1) * 128], lhsT=XB[:, sl, t, kc * 128:(kc + 1) * 128], rhs=IDENT[:],
                                start=True, stop=True), reads=[('XB', sl), 'IDENT'], writes=[('ps', b)])
                        copy_op(evac_eng(), dstT[:, kc, tg * 512:(tg + 1) * 512], ps[:, b, :],
                                reads=[('ps', b)], writes=[(tag, kc, tg)])
                T.barrier()

        def proj_fm(WTt, col0, rhsT, nkc, tg, b):
            for kc in range(nkc):
                T.op('pe', lambda e, kc=kc: e.matmul(ps[:, b, :], lhsT=WTt[:, kc, col0:col0 + 128],
                                                     rhs=rhsT[:, kc, tg * 512:(tg + 1) * 512],
                                                     start=(kc == 0), stop=(kc == nkc - 1)),
                     reads=['WT_any', 'RHS_any'], writes=[('ps', b)])

        def layer_norm_tile(Yt, ykey, LNP, out_ap, okey, st_tiles):
            STt, MVt, RSTDt = st_tiles
            for h in range(2):
                T.op('dve', lambda e, h=h: e.bn_stats(out=STt[:, h, :], in_=Yt[:, h * 512:(h + 1) * 512]),
                     reads=[ykey], writes=['LNST'])
            T.op('dve', lambda e: e.bn_aggr(out=MVt[:], in_=STt[:].rearrange("p a b -> p (a b)")), reads=['LNST'], writes=['LNMV'])
            T.op('act', lambda e: e.activation(out=RSTDt[:], in_=MVt[:, 1:2], func=AF.Sqrt, bias=EPSB[:, 0:1], scale=1.0),
                 reads=['LNMV'], writes=['LNRS'])
            T.op('dve', lambda e: e.reciprocal(RSTDt[:], RSTDt[:]), reads=['LNRS'], writes=['LNRS'])
            T.op('dve', lambda e: e.tensor_scalar(out=Yt[:], in0=Yt[:], scalar1=MVt[:, 0:1], scalar2=RSTDt[:, 0:1],
                                                  op0=ALU.subtract, op1=ALU.mult), reads=[ykey, 'LNMV', 'LNRS'], writes=[ykey])
            T.op('pool', lambda e: e.tensor_tensor(out=Yt[:], in0=Yt[:], in1=LNP[:, 0, :], op=ALU.mult), reads=[ykey, 'LNP'], writes=[ykey])
            T.op('pool', lambda e: e.tensor_tensor(out=out_ap, in0=Yt[:], in1=LNP[:, 1, :], op=ALU.add), reads=[ykey, 'LNP'], writes=[okey])

        def mixer(seq, li, xin_d):
            L = li
            tok0 = seq * S
            for j in range(4):
                T.dma('sp', CW[:, j, :], conv_w[L, j].rearrange("(n p) -> p n", p=128), 'prm', writes=['PRM'], slow=True)
            T.dma('sp', CB[:], conv_b[L].rearrange("(n p) -> p n", p=128), 'prm', writes=['PRM'], slow=True)
            T.dma('sp', BA[:], lru_ba[L].rearrange("(n p) -> p n", p=128), 'prm', writes=['PRM'], slow=True)
            T.dma('sp', BX[:], lru_bx[L].rearrange("(n p) -> p n", p=128), 'prm', writes=['PRM'], slow=True)
            T.dma('sp', LAM[:], lru_lam[L].rearrange("(n p) -> p n", p=128), 'prm', writes=['PRM'], slow=True)
            T.op('act', lambda e: e.activation(out=CL[:], in_=LAM[:], func=AF.Exp, scale=-1.0), reads=['PRM'], writes=['CL'])
            T.op('act', lambda e: e.activation(out=CL[:], in_=CL[:], func=AF.Ln, bias=1.0), reads=['CL'], writes=['CL'])
            T.op('dve', lambda e: e.tensor_scalar(out=CL2[:], in0=CL[:], scalar1=-16.0, scalar2=None, op0=ALU.mult), reads=['CL'], writes=['CL2'])
            T.op('dve', lambda e: e.tensor_scalar(out=CL[:], in0=CL[:], scalar1=-8.0, scalar2=None, op0=ALU.mult), reads=['CL', 'CL2'], writes=['CL'])

            chk('params')
            build_xt(None, XT, xin_d, NKC, 'XT')
            chk('xt')

            with ExitStack() as mst:
                QT = sb(mst, "QT", [128, NKC, S], BF16)
                with ExitStack() as ast:
                    WT = sb(ast, "WTa", [128, 2, NKC, 512], BF16)
                    KTE = sb(ast, "KTE", [128, S], BF16)
                    KTO = sb(ast, "KTO", [128, S], BF16)
                    KITE = sb(ast, "KITE", [128, S], BF16)
                    KITO = sb(ast, "KITO", [128, S], BF16)
                    QIT = sb(ast, "QIT", [128, 4, S], BF16)
                    V1E = sb(ast, "V1E", [128, NB, 128], BF16)
                    V1O = sb(ast, "V1O", [128, NB, 128], BF16)
                    WI = sb(ast, "WI", [128, NB, 8], F32)
                    SC = sb(ast, "SC", [128, S], F32)
                    BIS = sb(ast, "BIS", [128, 8], F32)
                    RELU = sb(ast, "RELU", [128, 2, 512], F32)
                    MASK = sb(ast, "MASK", [128, 2, S], BF16)
                    PT = sb(ast, "PT", [128, 2, 2048], BF16)
                    RS = sb(ast, "RS", [128, 2, 512], F32)

                    T.op('pool', lambda e: e.memset(KTE[:], 0.0), writes=['KTE'])
                    T.op('pool', lambda e: e.memset(KTO[:], 0.0), writes=['KTO'])
                    T.op('pool', lambda e: e.memset(KITE[:], 0.0), writes=['KITE'])
                    T.op('pool', lambda e: e.memset(KITO[:], 0.0), writes=['KITO'])
                    T.op('pool', lambda e: e.memset(V1E[:], 1.0), writes=['V1E'])
                    T.op('pool', lambda e: e.memset(V1O[:], 1.0), writes=['V1O'])

                    wslot = [0]

                    def wt_load(cols, width):
                        s_ = wslot[0] % 2
                        wslot[0] += 1
                        load_w(WT[:, s_, :, 0:width], w_in[L][:, cols:cols + width], 'wa%d' % s_, ('WT', s_))
                        return s_

                    def proj(s_, col0, tg, b):
                        for kc in range(NKC):
                            T.op('pe', lambda e, kc=kc: e.matmul(ps[:, b, :], lhsT=WT[:, s_, kc, col0:col0 + 128],
                                                                 rhs=XT[:, kc, tg * 512:(tg + 1) * 512],
                                                                 start=(kc == 0), stop=(kc == NKC - 1)),
                                 reads=[('WT', s_)], writes=[('ps', b)])

                    for hf in range(2):
                        s_ = wt_load(OFF['q'] + hf * 512, 512)
                        for cl in range(4):
                            c = hf * 4 + cl
                            for tg in range(4):
                                b = next_bank()
                                proj(s_, cl * 128, tg, b)
                                copy_op(evac_eng(), QT[:, c, tg * 512:(tg + 1) * 512], ps[:, b, :],
                                        reads=[('ps', b)], writes=[('QTc', c, tg)])
                    chk('projq')
                    s_ = wt_load(OFF['qi'], 512)
                    for c in range(4):
                        for tg in range(4):
                            b = next_bank()
                            proj(s_, c * 128, tg, b)
                            copy_op(evac_eng(), QIT[:, c, tg * 512:(tg + 1) * 512], ps[:, b, :],
                                    reads=[('ps', b)], writes=[('QIT', c, tg)])
                    chk('projqi')
                    s_ = wslot[0] % 2
                    wslot[0] += 1
                    wl = w_in[L]
                    for (dst0, src0, wd) in ((0, OFF['k'], 64), (64, OFF['k'], 64), (128, OFF['ki'], 64), (192, OFF['ki'], 64),
                                             (256, OFF['v'], 64), (320, OFF['wi'], 8)):
                        load_w(WT[:, s_, :, dst0:dst0 + wd], wl[:, src0:src0 + wd], 'wa%d' % s_, ('WT', s_))
                    for tg in range(4):
                        b = next_bank()
                        proj(s_, 0, tg, b)
                        copy_op('act', KTE[0:64, tg * 512:(tg + 1) * 512], ps[0:64, b, :], reads=[('ps', b)], writes=['KTE'])
                        copy_op('act', KTO[64:128, tg * 512:(tg + 1) * 512], ps[64:128, b, :], reads=[('ps', b)], writes=['KTO', ('ps', b)])
                        b = next_bank()
                        proj(s_, 128, tg, b)
                        copy_op('act', KITE[0:64, tg * 512:(tg + 1) * 512], ps[0:64, b, :], reads=[('ps', b)], writes=['KITE'])
                        copy_op('act', KITO[64:128, tg * 512:(tg + 1) * 512], ps[64:128, b, :], reads=[('ps', b)], writes=['KITO', ('ps', b)])
                    chk('projk')
                    for t4 in range(4):
                        b = next_bank()
                        for tl in range(4):
                            t = t4 * 4 + tl
                            for kc in range(NKC):
                                T.op('pe', lambda e, kc=kc, t=t, tl=tl: e.matmul(
                                    ps[:, b, tl * 128:tl * 128 + 72], lhsT=XT[:, kc, t * 128:(t + 1) * 128],
                                    rhs=WT[:, s_, kc, 256:328], start=(kc == 0), stop=(kc == NKC - 1)),
                                     reads=[('WT', s_)], writes=[('ps', b)])
                        pv = ps[:, b, :].rearrange("p (a c) -> p a c", a=4)
                        copy_op('act', V1E[:, t4 * 4:(t4 + 1) * 4, 0:64], pv[:, :, 0:64], reads=[('ps', b)], writes=['V1E'])
                        copy_op('act', WI[:, t4 * 4:(t4 + 1) * 4, :], pv[:, :, 64:72], reads=[('ps', b)], writes=['WI', ('ps', b)])
                        copy_op('pool', V1O[:, t4 * 4:(t4 + 1) * 4, 64:128], V1E[:, t4 * 4:(t4 + 1) * 4, 0:64], reads=['V1E'], writes=['V1O'])
                    T.barrier()

                    chk('proj')

                    bank_list[0] = [0, 1, 2, 3]

                    def score(j):
                        nk = 128 * (j + 1)
                        ms = j % 2
                        if j < 2:
                            return
                        nsl = (nk + 511) // 512
                        for h in range(8):
                            c, par = h // 2, h % 2
                            KI = KITE if par == 0 else KITO
                            for si in range(nsl):
                                k0 = si * 512
                                k1 = min(nk, k0 + 512)
                                b = next_bank()
                                T.op('pe', lambda e, b=b, c=c, KI=KI, k0=k0, k1=k1: e.matmul(
                                    ps[:, b, 0:k1 - k0], lhsT=QIT[:, c, j * 128:(j + 1) * 128], rhs=KI[:, k0:k1],
                                    start=True, stop=True), reads=[], writes=[('ps', b)])
                                rs_ = (h * nsl + si) % 2
                                T.op('act', lambda e, b=b, rs_=rs_, k0=k0, k1=k1: e.activation(
                                    out=RELU[:, rs_, 0:k1 - k0], in_=ps[:, b, 0:k1 - k0], func=AF.Relu),
                                     reads=[('ps', b)], writes=[('RELU', rs_)])
                                if h == 0:
                                    T.op('dve', lambda e, rs_=rs_, k0=k0, k1=k1, h=h: e.tensor_scalar(
                                        out=SC[:, k0:k1], in0=RELU[:, rs_, 0:k1 - k0], scalar1=WI[:, j, h:h + 1], scalar2=None,
                                        op0=ALU.mult), reads=[('RELU', rs_)], writes=[('SC', si)])
                                else:
                                    T.op('dve', lambda e, rs_=rs_, k0=k0, k1=k1, h=h: e.scalar_tensor_tensor(
                                        out=SC[:, k0:k1], in0=RELU[:, rs_, 0:k1 - k0], scalar=WI[:, j, h:h + 1], in1=SC[:, k0:k1],
                                        op0=ALU.mult, op1=ALU.add), reads=[('RELU', rs_), ('SC', si)], writes=[('SC', si)])
                        allsc = [('SC', si) for si in range(nsl)]
                        T.op('dve', lambda e: e.tensor_reduce(out=BIS[:, 0:1], in_=SC[:, 0:nk], axis=mybir.AxisListType.X, op=ALU.min),
                             reads=allsc, writes=['BLO'])
                        T.op('dve', lambda e: e.tensor_reduce(out=BIS[:, 1:2], in_=SC[:, 0:nk], axis=mybir.AxisListType.X, op=ALU.max),
                             reads=allsc, writes=['BRG'])
                        T.op('dve', lambda e: e.tensor_tensor(out=BIS[:, 1:2], in0=BIS[:, 1:2], in1=BIS[:, 0:1], op=ALU.subtract),
                             reads=['BLO', 'BRG'], writes=['BRG'])
                        T.op('dve', lambda e: e.tensor_tensor(out=SC[:, j * 128:nk], in0=SC[:, j * 128:nk], in1=ADMNEG[:], op=ALU.add),
                             reads=allsc, writes=allsc)
                        for r in range(1, NBIS + 1):
                            sc_ = float(2.0 ** -r)
                            T.op('dve', lambda e: e.scalar_tensor_tensor(out=BIS[:, 2:3], in0=BIS[:, 1:2], scalar=sc_, in1=BIS[:, 0:1],
                                                                         op0=ALU.mult, op1=ALU.add), reads=['BLO', 'BRG'], writes=['BMID'])
                            T.op('dve', lambda e: e.tensor_scalar(out=MASK[:, ms, 0:nk], in0=SC[:, 0:nk], scalar1=BIS[:, 2:3], scalar2=0.0,
                                                                  op0=ALU.is_ge, op1=ALU.add, accum_out=BIS[:, 3:4]),
                                 reads=allsc + ['BMID'], writes=[('MASK', ms), 'BCNT'])
                            T.op('dve', lambda e: e.tensor_scalar(out=BIS[:, 4:5], in0=BIS[:, 3:4], scalar1=255.5, scalar2=sc_,
                                                                  op0=ALU.is_ge, op1=ALU.mult), reads=['BCNT'], writes=['BTT'])
                            T.op('dve', lambda e: e.scalar_tensor_tensor(out=BIS[:, 0:1], in0=BIS[:, 4:5], scalar=BIS[:, 1:2], in1=BIS[:, 0:1],
                                                                         op0=ALU.mult, op1=ALU.add), reads=['BTT', 'BRG', 'BLO'], writes=['BLO'])
                        T.op('dve', lambda e: e.tensor_scalar(out=MASK[:, ms, 0:nk], in0=SC[:, 0:nk], scalar1=BIS[:, 0:1], scalar2=-1.0,
                                                              op0=ALU.is_ge, op1=ALU.add), reads=allsc + ['BLO'], writes=[('MASK', ms)])

                    pt_ctr = [0]

                    def attend(j):
                        ms = j % 2
                        q0 = j * 128
                        for kb in range(j + 1):
                            near = j - kb
                            pslot = pt_ctr[0] % 2
                            pt_ctr[0] += 1
                            for g in range(4):
                                c0 = (g // 2) * 4
                                KT = KTE if g % 2 == 0 else KTO
                                V1 = V1E if g % 2 == 0 else V1O
                                b = next_bank()
                                has_mask = (j >= 2) or (kb == j)
                                has_bias = near <= 1
                                T.op('pe', lambda e, b=b, KT=KT, c0=c0: e.matmul(
                                    ps[:, b, :].rearrange("p (a c) -> p a c", a=4), lhsT=KT[:, kb * 128:(kb + 1) * 128],
                                    rhs=QT[:, c0:c0 + 4, q0:q0 + 128], start=True, stop=not (has_mask or has_bias)),
                                     reads=[('QTb', j)], writes=[('ps', b)])
                                if has_mask:
                                    if j >= 2:
                                        ml = MASK[:, ms, kb * 128:(kb + 1) * 128]
                                        rd = [('MASK', ms)]
                                    else:
                                        ml = ADM01[:]
                                        rd = []
                                    T.op('pe', lambda e, b=b, ml=ml: e.matmul(ps[:, b, :], lhsT=ml, rhs=I4[:], start=False, stop=not has_bias),
                                         reads=rd, writes=[('ps', b)])
                                if has_bias:
                                    T.op('pe', lambda e, b=b, g=g: e.matmul(ps[:, b, :], lhsT=IDENT[:], rhs=BIAS[:, near, g * 512:(g + 1) * 512],
                                                                            start=False, stop=True), reads=[], writes=[('ps', b)])
                                T.op('act', lambda e, b=b, g=g: e.activation(out=PT[:, pslot, g * 512:(g + 1) * 512], in_=ps[:, b, :],
                                                                             func=AF.Exp, scale=0.125),
                                     reads=[('ps', b)], writes=[('PT', pslot, g)])
                                T.op('pe', lambda e, g=g, V1=V1: e.matmul(ps[:, 4 + g, :], lhsT=V1[:, kb, :], rhs=PT[:, pslot, g * 512:(g + 1) * 512],
                                                                          start=(kb == 0), stop=(kb == j)),
                                     reads=[('PT', pslot, g)], writes=[('oacc', g)])
                        for g in range(4):
                            c0 = (g // 2) * 4
                            rsl = g % 2
                            if g % 2 == 0:
                                T.op('act', lambda e, g=g: e.activation(out=RS[0:64, rsl, :], in_=ps[64:128, 4 + g, :], func=AF.Ln),
                                     reads=[('oacc', g)], writes=[('RS', rsl)])
                                T.op('act', lambda e, g=g: e.activation(out=RS[0:64, rsl, :], in_=RS[0:64, rsl, :], func=AF.Exp, scale=-1.0),
                                     reads=[('RS', rsl)], writes=[('RS', rsl)])
                                T.op('dve', lambda e, g=g: e.tensor_tensor(
                                    out=QT[0:64, c0:c0 + 4, q0:q0 + 128], in0=ps[0:64, 4 + g, :].rearrange("p (a c) -> p a c", a=4),
                                    in1=RS[0:64, rsl, :].rearrange("p (a c) -> p a c", a=4), op=ALU.mult),
                                     reads=[('oacc', g), ('RS', rsl)], writes=[('oacc', g), ('QTb', j)])
                            else:
                                T.op('act', lambda e, g=g: e.activation(out=RS[64:128, rsl, :], in_=ps[0:64, 4 + g, :], func=AF.Ln),
                                     reads=[('oacc', g)], writes=[('RS', rsl)])
                                T.op('act', lambda e, g=g: e.activation(out=RS[64:128, rsl, :], in_=RS[64:128, rsl, :], func=AF.Exp, scale=-1.0),
                                     reads=[('RS', rsl)], writes=[('RS', rsl)])
                                T.op('dve', lambda e, g=g: e.tensor_tensor(
                                    out=QT[64:128, c0:c0 + 4, q0:q0 + 128], in0=ps[64:128, 4 + g, :].rearrange("p (a c) -> p a c", a=4),
                                    in1=RS[64:128, rsl, :].rearrange("p (a c) -> p a c", a=4), op=ALU.mult),
                                     reads=[('oacc', g), ('RS', rsl)], writes=[('oacc', g), ('QTb', j)])

                    score(0)
                    for j in range(NB):
                        if j + 1 < NB:
                            score(j + 1)
                        attend(j)
                    T.barrier()
                    bank_list[0] = list(range(8))
                if dbg and 'oT' in dbg:
                    with ExitStack() as dst_:
                        DT_ = sb(dst_, "DBGT", [128, S], F32)
                        for c in range(NKC):
                            T.op('dve', lambda e, c=c: e.tensor_copy(DT_[:], QT[:, c, :]), reads=['dbgd'], writes=['dbgt'])
                            T.dma('sp', dbg_out['oT'][c * 128:(c + 1) * 128, :], DT_[:], 'dbg', reads=['dbgt'], writes=['dbgd'])
                        T.barrier()

                MT = sb(mst, "MT", [128, NKC, S], BF16)

                def branch(WBR, gate_off, first):
                    with ExitStack() as bst:
                        WTb = sb(bst, "WTb", [128, 4, NKC, 512], BF16)
                        SGt = sb(bst, "SGt", [128, 2, 512], F32)
                        TMPt = sb(bst, "TMPt", [128, 2, 512], F32)
                        k_ = [0]
                        for hf in range(2):
                            sA = (hf * 2) % 4
                            sG = (hf * 2 + 1) % 4
                            load_w(WTb[:, sA, :, :], WBR[L][:, hf * 512:(hf + 1) * 512], 'wb%d' % sA, ('WTb', sA))
                            load_w(WTb[:, sG, :, :], w_in[L][:, gate_off + hf * 512:gate_off + (hf + 1) * 512], 'wb%d' % sG, ('WTb', sG))
                            for cl in range(4):
                                fo = hf * 4 + cl
                                for tg in range(4):
                                    bA = next_bank()
                                    for kc in range(NKC):
                                        T.op('pe', lambda e, kc=kc: e.matmul(ps[:, bA, :], lhsT=WTb[:, sA, kc, cl * 128:(cl + 1) * 128],
                                                                             rhs=QT[:, kc, tg * 512:(tg + 1) * 512],
                                                                             start=(kc == 0), stop=(kc == NKC - 1)),
                                             reads=[('WTb', sA), 'QTall'], writes=[('ps', bA)])
                                    bG = next_bank()
                                    for kc in range(NKC):
                                        T.op('pe', lambda e, kc=kc: e.matmul(ps[:, bG, :], lhsT=WTb[:, sG, kc, cl * 128:(cl + 1) * 128],
                                                                             rhs=XT[:, kc, tg * 512:(tg + 1) * 512],
                                                                             start=(kc == 0), stop=(kc == NKC - 1)),
                                             reads=[('WTb', sG)], writes=[('ps', bG)])
                                    sl = k_[0] % 2
                                    k_[0] += 1
                                    T.op('act', lambda e: e.activation(out=SGt[:, sl, :], in_=ps[:, bG, :], func=AF.Sigmoid),
                                         reads=[('ps', bG)], writes=[('SG', sl)])
                                    if first:
                                        T.op('dve', lambda e: e.tensor_tensor(out=MT[:, fo, tg * 512:(tg + 1) * 512], in0=ps[:, bA, :],
                                                                              in1=SGt[:, sl, :], op=ALU.mult),
                                             reads=[('ps', bA), ('SG', sl)], writes=[('ps', bA), ('MT', fo, tg)])
                                    else:
                                        T.op('dve', lambda e: e.tensor_tensor(out=TMPt[:, sl, :], in0=ps[:, bA, :], in1=SGt[:, sl, :], op=ALU.mult),
                                             reads=[('ps', bA), ('SG', sl)], writes=[('ps', bA), ('TMP', sl)])
                                        T.op('pool', lambda e: e.tensor_tensor(out=MT[:, fo, tg * 512:(tg + 1) * 512],
                                                                               in0=MT[:, fo, tg * 512:(tg + 1) * 512], in1=TMPt[:, sl, :], op=ALU.add),
                                             reads=[('TMP', sl), ('MT', fo, tg)], writes=[('MT', fo, tg)])
                        T.barrier()

                chk('attn')
                branch(w_br_attn, OFF['ga'], True)
                chk('brA')

                with ExitStack() as rst:
                    WTr = sb(rst, "WTr", [128, 2, NKC, 512], BF16)
                    WGA = sb(rst, "WGA", [128, 8, 128], BF16)
                    WGX = sb(rst, "WGX", [128, 8, 128], BF16)
                    NR = 3
                    XR = sb(rst, "XR", [128, NR, 3 + 512], F32)
                    XC = sb(rst, "XC", [128, NR, 512], F32)
                    XCB = sb(rst, "XCB", [128, NR, 512], BF16)
                    RA = sb(rst, "RA", [128, NR, 512], F32)
                    SQ = sb(rst, "SQ", [128, NR, 512], F32)
                    IU = sb(rst, "IU", [128, NR, 512], F32)
                    HH = sb(rst, "HH", [128, NR, 512], F32)
                    GG = sb(rst, "GG", [128, NR, 512], F32)
                    T.dma('pool', WGA[:], lru_wa[L].rearrange("n c d -> c n d"), 'wg', writes=['WGA'])
                    T.dma('pool', WGX[:], lru_wx[L].rearrange("n c d -> c n d"), 'wg', writes=['WGX'])
                    iters = [(hf, cl, tg) for hf in range(2) for cl in range(4) for tg in range(4)]

                    def stage1(it):
                        hf, cl, tg = iters[it]
                        n = hf * 4 + cl
                        r_ = it % NR
                        rp = (it - 1) % NR
                        K = lambda nm: (nm, r_)
                        tsl = slice(tg * 512, (tg + 1) * 512)
                        if cl == 0 and tg == 0:
                            load_w(WTr[:, 0, :, :], w_in[L][:, OFF['xr'] + hf * 512:OFF['xr'] + (hf + 1) * 512], 'wr0', ('WTr', 0))
                        bX = next_bank()
                        for kc in range(NKC):
                            T.op('pe', lambda e, kc=kc: e.matmul(ps[:, bX, :], lhsT=WTr[:, 0, kc, cl * 128:(cl + 1) * 128], rhs=XT[:, kc, tsl],
                                                                 start=(kc == 0), stop=(kc == NKC - 1)), reads=[('WTr', 0)], writes=[('ps', bX)])
                        if tg == 0:
                            T.op('pool', lambda e: e.memset(XR[:, r_, 0:3], 0.0), writes=[K('XR')])
                        else:
                            T.op('pool', lambda e: e.tensor_copy(XR[:, r_, 0:3], XR[:, rp, 512:515]), reads=[('XR', rp)], writes=[K('XR')])
                        T.op('act', lambda e: e.activation(out=XR[:, r_, 3:515], in_=ps[:, bX, :], func=AF.Copy), reads=[('ps', bX)], writes=[K('XR')])
                        T.op('dve', lambda e: e.tensor_scalar(out=XC[:, r_, :], in0=XR[:, r_, 0:512], scalar1=CW[:, 0, n:n + 1], scalar2=CB[:, n:n + 1],
                                                              op0=ALU.mult, op1=ALU.add), reads=[K('XR'), 'PRM'], writes=[K('XC')])
                        for jt in range(1, 4):
                            T.op('dve', lambda e, jt=jt: e.scalar_tensor_tensor(out=XC[:, r_, :], in0=XR[:, r_, jt:jt + 512], scalar=CW[:, jt, n:n + 1],
                                                                               in1=XC[:, r_, :], op0=ALU.mult, op1=ALU.add),
                                 reads=[K('XR'), K('XC')], writes=[K('XC')])
                        T.op('pool', lambda e: e.tensor_copy(XCB[:, r_, :], XC[:, r_, :]), reads=[K('XC')], writes=[K('XCB')])

                    def stage2(it):
                        hf, cl, tg = iters[it]
                        n = hf * 4 + cl
                        r_ = it % NR
                        rp = (it - 1) % NR
                        K = lambda nm: (nm, r_)
                        tsl = slice(tg * 512, (tg + 1) * 512)
                        if cl == 0 and tg == 0:
                            load_w(WTr[:, 1, :, :], w_in[L][:, OFF['yr'] + hf * 512:OFF['yr'] + (hf + 1) * 512], 'wr1', ('WTr', 1))
                        bR = next_bank()
                        T.op('pe', lambda e: e.matmul(ps[:, bR, :], lhsT=WGA[:, n, :], rhs=XCB[:, r_, :], start=True, stop=True),
                             reads=[K('XCB'), 'WGA'], writes=[('ps', bR)])
                        bI = next_bank()
                        T.op('pe', lambda e: e.matmul(ps[:, bI, :], lhsT=WGX[:, n, :], rhs=XCB[:, r_, :], start=True, stop=True),
                             reads=[K('XCB'), 'WGX'], writes=[('ps', bI)])
                        bY = next_bank()
                        for kc in range(NKC):
                            T.op('pe', lambda e, kc=kc: e.matmul(ps[:, bY, :], lhsT=WTr[:, 1, kc, cl * 128:(cl + 1) * 128], rhs=XT[:, kc, tsl],
                                                                 start=(kc == 0), stop=(kc == NKC - 1)), reads=[('WTr', 1)], writes=[('ps', bY)])
                        T.op('act', lambda e: e.activation(out=RA[:, r_, :], in_=ps[:, bR, :], func=AF.Sigmoid, bias=BA[:, n:n + 1]),
                             reads=[('ps', bR), 'PRM'], writes=[K('RA')])
                        T.op('act', lambda e: e.activation(out=IU[:, r_, :], in_=ps[:, bI, :], func=AF.Sigmoid, bias=BX[:, n:n + 1]),
                             reads=[('ps', bI), 'PRM'], writes=[K('IU')])
                        T.op('act', lambda e: e.activation(out=GG[:, r_, :], in_=ps[:, bY, :], func=AF.Gelu_apprx_tanh),
                             reads=[('ps', bY)], writes=[K('GG')])
                        T.op('act', lambda e: e.activation(out=SQ[:, r_, :], in_=RA[:, r_, :], func=AF.Exp, scale=CL2[:, n:n + 1]),
                             reads=[K('RA'), 'CL2'], writes=[K('SQ')])
                        T.op('act', lambda e: e.activation(out=RA[:, r_, :], in_=RA[:, r_, :], func=AF.Exp, scale=CL[:, n:n + 1]),
                             reads=[K('RA'), 'CL'], writes=[K('RA')])
                        T.op('act', lambda e: e.activation(out=SQ[:, r_, :], in_=SQ[:, r_, :], func=AF.Sqrt, scale=-1.0, bias=1.0),
                             reads=[K('SQ')], writes=[K('SQ')])
                        T.op('pool', lambda e: e.tensor_tensor(out=IU[:, r_, :], in0=IU[:, r_, :], in1=XC[:, r_, :], op=ALU.mult),
                             reads=[K('IU'), K('XC')], writes=[K('IU')])
                        T.op('pool', lambda e: e.tensor_tensor(out=IU[:, r_, :], in0=IU[:, r_, :], in1=SQ[:, r_, :], op=ALU.mult),
                             reads=[K('IU'), K('SQ')], writes=[K('IU')])
                        init = 0.0 if tg == 0 else HH[:, rp, 511:512]
                        T.op('dve', lambda e: e.tensor_tensor_scan(out=HH[:, r_, :], data0=RA[:, r_, :], data1=IU[:, r_, :], initial=init,
                                                                   op0=ALU.mult, op1=ALU.add),
                             reads=[K('RA'), K('IU'), ('HH', rp)], writes=[K('HH')])
                        T.op('pool', lambda e: e.tensor_tensor(out=QT[:, n, tsl], in0=HH[:, r_, :], in1=GG[:, r_, :], op=ALU.mult),
                             reads=[K('HH'), K('GG')], writes=[('ORT', n, tg)])

                    NI = len(iters)
                    for k in range(NI + 1):
                        if k < NI:
                            stage1(k)
                        if k >= 1:
                            stage2(k - 1)
                    T.barrier()
                if dbg and 'orT' in dbg:
                    with ExitStack() as dst_:
                        DT_ = sb(dst_, "DBGT2", [128, S], F32)
                        for c in range(NKC):
                            T.op('dve', lambda e, c=c: e.tensor_copy(DT_[:], QT[:, c, :]), reads=['dbgd'], writes=['dbgt'])
                            T.dma('sp', dbg_out['orT'][c * 128:(c + 1) * 128, :], DT_[:], 'dbg', reads=['dbgt'], writes=['dbgd'])
                        T.barrier()

                chk('rnn')
                branch(w_br_rnn, OFF['gr'], False)
                chk('brB')

                with ExitStack() as cst:
                    WO = sb(cst, "WO", [128, NKC, D], BF16)
                    XI = sb(cst, "XI", [128, 2, D], F32)
                    YT = sb(cst, "YT", [128, 2, D], F32)
                    LNP = sb(cst, "LNP", [128, 2, D], F32)
                    STt = sb(cst, "STt", [128, 2, 6], F32)
                    MVt = sb(cst, "MVt", [128, 2], F32)
                    RSt = sb(cst, "RSt", [128, 1], F32)
                    for hf in range(2):
                        load_w(WO[:, :, hf * 512:(hf + 1) * 512], w_o[L][:, hf * 512:(hf + 1) * 512], 'wo', 'WO')
                    T.dma('sp', LNP[:, 0, :], ln1_g[L:L + 1, :].broadcast_to([128, D]), 'lnp', writes=['LNP'])
                    T.dma('sp', LNP[:, 1, :], ln1_b[L:L + 1, :].broadcast_to([128, D]), 'lnp', writes=['LNP'])
                    for t in range(NB):
                        sl = t % 2
                        T.dma('sp', XI[:, sl, :], xin_d[t * 128:(t + 1) * 128, :], 'xi%d' % sl, writes=[('XI', sl)])
                        for hf in range(2):
                            b = next_bank()
                            for kc in range(NKC):
                                T.op('pe', lambda e, kc=kc: e.matmul(ps[:, b, :], lhsT=MT[:, kc, t * 128:(t + 1) * 128], rhs=WO[:, kc, hf * 512:(hf + 1) * 512],
                                                                     start=(kc == 0), stop=(kc == NKC - 1)), reads=['WO'], writes=[('ps', b)])
                            T.op('dve', lambda e, hf=hf, b=b: e.scalar_tensor_tensor(out=YT[:, sl, hf * 512:(hf + 1) * 512], in0=XI[:, sl, hf * 512:(hf + 1) * 512],
                                                                                    scalar=ALPHA, in1=ps[:, b, :], op0=ALU.mult, op1=ALU.add),
                                 reads=[('XI', sl), ('ps', b)], writes=[('YT', sl), ('ps', b)])
                        layer_norm_tile(YT[:, sl, :], ('YT', sl), LNP, YT[:, sl, :], ('YT', sl), (STt, MVt, RSt))
                        T.dma('sp', x1s_d[t * 128:(t + 1) * 128, :], YT[:, sl, :], 'x1o%d' % sl, reads=[('YT', sl)], writes=[('x1s', t)])
                    T.barrier()

        def ffn(seq, li, xout_d):
            L = li
            jf = li // 2
            moe = (li % 2 == 1)
            tok0 = seq * S
            build_xt(None, XT, x1s_d, NKC, 'XT')
            with ExitStack() as fst:
                ACC = sb(fst, "ACC", [128, NB, D], F32)
                CMB = sb(fst, "CMB", [128, NB, 8], F32)
                with ExitStack() as pst:
                    PTT = sb(pst, "PTT", [128, 2, S], BF16)
                    WPG = sb(pst, "WPG", [128, NKC, D], BF16)
                    WPP = sb(pst, "WPP", [128, 2, D], BF16)
                    WR = sb(pst, "WR", [128, NKC, 8], BF16)
                    SGt = sb(pst, "SGt2", [128, 2, 512], F32)
                    TMPt = sb(pst, "TMPt2", [128, 2, 512], F32)
                    LG = sb(pst, "LG", [128, 8], F32)
                    M8r = sb(pst, "M8r", [128, 8], F32)
                    GT = sb(pst, "GT", [128, 4], F32)
                    T1 = sb(pst, "T1", [128, 8], F32)
                    build_xt(None, PTT, p_in[L][tok0:tok0 + S, :], 2, 'PTT')
                    for hf in range(2):
                        load_w(WPG[:, :, hf * 512:(hf + 1) * 512], ple_w_gate[L][:, hf * 512:(hf + 1) * 512], 'wpg', 'WPG')
                        load_w(WPP[:, :, hf * 512:(hf + 1) * 512], ple_w_proj[L][:, hf * 512:(hf + 1) * 512], 'wpp', 'WPP')
                    if moe:
                        load_w(WR[:], moe_router[jf], 'wr', 'WR')
                        T.dma('sp', RB[:], moe_router_b[jf:jf + 1, :].broadcast_to([128, 8]), 'prm2', writes=['RB'])
                    k_ = [0]
                    for t in range(NB):
                        T.dma('sp', ACC[:, t, :], x1s_d[t * 128:(t + 1) * 128, :], 'acc%d' % (t % 4), writes=[('ACC', t)])
                        T.op('pool', lambda e, t=t: e.tensor_scalar(out=ACC[:, t, :], in0=ACC[:, t, :], scalar1=ALPHA, scalar2=None, op0=ALU.mult),
                             reads=[('ACC', t)], writes=[('ACC', t)])
                        for hf in range(2):
                            bG = next_bank()
                            for kc in range(NKC):
                                T.op('pe', lambda e, kc=kc: e.matmul(ps[:, bG, :], lhsT=XT[:, kc, t * 128:(t + 1) * 128], rhs=WPG[:, kc, hf * 512:(hf + 1) * 512],
                                                                     start=(kc == 0), stop=(kc == NKC - 1)), reads=['WPG'], writes=[('ps', bG)])
                            bP = next_bank()
                            for kc in range(2):
                                T.op('pe', lambda e, kc=kc: e.matmul(ps[:, bP, :], lhsT=PTT[:, kc, t * 128:(t + 1) * 128], rhs=WPP[:, kc, hf * 512:(hf + 1) * 512],
                                                                     start=(kc == 0), stop=(kc == 1)), reads=['WPP'], writes=[('ps', bP)])
                            sl = k_[0] % 2
                            k_[0] += 1
                            T.op('act', lambda e: e.activation(out=SGt[:, sl, :], in_=ps[:, bG, :], func=AF.Sigmoid), reads=[('ps', bG)], writes=[('SG', sl)])
                            T.op('dve', lambda e: e.tensor_tensor(out=TMPt[:, sl, :], in0=ps[:, bP, :], in1=SGt[:, sl, :], op=ALU.mult),
                                 reads=[('ps', bP), ('SG', sl)], writes=[('ps', bP), ('TMP', sl)])
                            T.op('pool', lambda e, t=t, hf=hf: e.tensor_tensor(out=ACC[:, t, hf * 512:(hf + 1) * 512], in0=ACC[:, t, hf * 512:(hf + 1) * 512],
                                                                               in1=TMPt[:, sl, :], op=ALU.add), reads=[('TMP', sl), ('ACC', t)], writes=[('ACC', t)])
                        if moe:
                            b = next_bank()
                            for kc in range(NKC):
                                T.op('pe', lambda e, kc=kc: e.matmul(ps[:, b, 0:8], lhsT=XT[:, kc, t * 128:(t + 1) * 128], rhs=WR[:, kc, :],
                                                                     start=(kc == 0), stop=(kc == NKC - 1)), reads=['WR'], writes=[('ps', b)])
                            T.op('dve', lambda e: e.tensor_tensor(out=LG[:], in0=ps[:, b, 0:8], in1=RB[:], op=ALU.add), reads=[('ps', b), 'RB'], writes=['LG', ('ps', b)])
                            T.op('dve', lambda e: e.max(out=M8r[:], in_=LG[:]), reads=['LG'], writes=['M8r'])
                            T.op('dve', lambda e: e.tensor_tensor(out=GT[:, 0:1], in0=M8r[:, 1:2], in1=M8r[:, 0:1], op=ALU.subtract), reads=['M8r'], writes=['GT0'])
                            T.op('act', lambda e: e.activation(out=GT[:, 1:2], in_=GT[:, 0:1], func=AF.Exp), reads=['GT0'], writes=['GT1'])
                            T.op('dve', lambda e: e.tensor_scalar(out=GT[:, 2:3], in0=GT[:, 1:2], scalar1=1.0, scalar2=None, op0=ALU.add),
                                 reads=['GT1'], writes=['GT2'])
                            T.op('dve', lambda e: e.reciprocal(GT[:, 2:3], GT[:, 2:3]), reads=['GT2'], writes=['GT2'])
                            T.op('dve', lambda e: e.tensor_tensor(out=GT[:, 3:4], in0=GT[:, 1:2], in1=GT[:, 2:3], op=ALU.mult), reads=['GT1', 'GT2'], writes=['GT3'])
                            T.op('dve', lambda e: e.tensor_scalar(out=T1[:], in0=LG[:], scalar1=M8r[:, 0:1], scalar2=GT[:, 2:3], op0=ALU.is_equal, op1=ALU.mult),
                                 reads=['LG', 'M8r', 'GT2'], writes=['T1'])
                            T.op('dve', lambda e, t=t: e.tensor_scalar(out=CMB[:, t, :], in0=LG[:], scalar1=M8r[:, 1:2], scalar2=GT[:, 3:4], op0=ALU.is_equal, op1=ALU.mult),
                                 reads=['LG', 'M8r', 'GT3'], writes=[('CMB', t)])
                            T.op('dve', lambda e, t=t: e.tensor_tensor(out=CMB[:, t, :], in0=CMB[:, t, :], in1=T1[:], op=ALU.add), reads=['T1', ('CMB', t)], writes=[('CMB', t)])
                    T.barrier()

                with ExitStack() as gst:
                    UW = 256
                    WGU = sb(gst, "WGU", [128, 2, 2, NKC, UW], BF16)
                    WDN = sb(gst, "WDN", [128, 2, UW // 128, D], BF16)
                    HT = sb(gst, "HT", [128, 2, UW // 128, S], BF16)
                    SLt = sb(gst, "SLt", [128, 2, 512], F32)
                    nexp = NEXP if moe else 1
                    dff = D_FFE if moe else D_FF
                    ui = [0]
                    k_ = [0]
                    for ex in range(nexp):
                        if moe:
                            wg_d, wu_d, wd_d = moe_w_gate[jf, ex], moe_w_up[jf, ex], moe_w_down[jf, ex]
                        else:
                            wg_d, wu_d, wd_d = ffn_w_gate[jf], ffn_w_up[jf], ffn_w_down[jf]
                        for u in range(dff // UW):
                            us = ui[0] % 2
                            ui[0] += 1
                            load_w(WGU[:, us, 0, :, :], wg_d[:, u * UW:(u + 1) * UW], 'wu%d' % us, ('WGU', us))
                            load_w(WGU[:, us, 1, :, :], wu_d[:, u * UW:(u + 1) * UW], 'wv%d' % us, ('WGU', us))
                            load_w(WDN[:, us, :, :], wd_d[u * UW:(u + 1) * UW, :], 'wd%d' % us, ('WDN', us))
                            for cl in range(UW // 128):
                                for tg in range(4):
                                    bg = next_bank()
                                    for kc in range(NKC):
                                        T.op('pe', lambda e, kc=kc: e.matmul(ps[:, bg, :], lhsT=WGU[:, us, 0, kc, cl * 128:(cl + 1) * 128],
                                                                             rhs=XT[:, kc, tg * 512:(tg + 1) * 512], start=(kc == 0), stop=(kc == NKC - 1)),
                                             reads=[('WGU', us)], writes=[('ps', bg)])
                                    bu = next_bank()
                                    for kc in range(NKC):
                                        T.op('pe', lambda e, kc=kc: e.matmul(ps[:, bu, :], lhsT=WGU[:, us, 1, kc, cl * 128:(cl + 1) * 128],
                                                                             rhs=XT[:, kc, tg * 512:(tg + 1) * 512], start=(kc == 0), stop=(kc == NKC - 1)),
                                             reads=[('WGU', us)], writes=[('ps', bu)])
                                    sl = k_[0] % 2
                                    k_[0] += 1
                                    T.op('act', lambda e: e.activation(out=SLt[:, sl, :], in_=ps[:, bg, :], func=AF.Silu), reads=[('ps', bg)], writes=[('SL', sl)])
                                    T.op('dve', lambda e: e.tensor_tensor(out=HT[:, us, cl, tg * 512:(tg + 1) * 512], in0=ps[:, bu, :], in1=SLt[:, sl, :], op=ALU.mult),
                                         reads=[('ps', bu), ('SL', sl)], writes=[('ps', bu), ('HT', us)])
                            for t in range(NB):
                                for hf in range(2):
                                    b = next_bank()
                                    ncl = UW // 128
                                    for cl in range(ncl):
                                        T.op('pe', lambda e, cl=cl: e.matmul(ps[:, b, :], lhsT=HT[:, us, cl, t * 128:(t + 1) * 128],
                                                                             rhs=WDN[:, us, cl, hf * 512:(hf + 1) * 512], start=(cl == 0), stop=(cl == ncl - 1)),
                                             reads=[('HT', us), ('WDN', us)], writes=[('ps', b)])
                                    if moe:
                                        T.op('dve', lambda e, t=t, hf=hf, b=b, ex=ex: e.scalar_tensor_tensor(
                                            out=ACC[:, t, hf * 512:(hf + 1) * 512], in0=ps[:, b, :], scalar=CMB[:, t, ex:ex + 1],
                                            in1=ACC[:, t, hf * 512:(hf + 1) * 512], op0=ALU.mult, op1=ALU.add),
                                             reads=[('ps', b), ('ACC', t, hf), ('CMB', t)], writes=[('ps', b), ('ACC', t, hf)])
                                    else:
                                        T.op('dve', lambda e, t=t, hf=hf, b=b: e.tensor_tensor(
                                            out=ACC[:, t, hf * 512:(hf + 1) * 512], in0=ps[:, b, :], in1=ACC[:, t, hf * 512:(hf + 1) * 512], op=ALU.add),
                                             reads=[('ps', b), ('ACC', t, hf)], writes=[('ps', b), ('ACC', t, hf)])
                    T.barrier()

                with ExitStack() as lst:
                    LNP = sb(lst, "LNP2", [128, 2, D], F32)
                    STt = sb(lst, "STt2", [128, 2, 6], F32)
                    MVt = sb(lst, "MVt2", [128, 2], F32)
                    RSt = sb(lst, "RSt2", [128, 1], F32)
                    T.dma('sp', LNP[:, 0, :], ln2_g[L:L + 1, :].broadcast_to([128, D]), 'lnp', writes=['LNP'])
                    T.dma('sp', LNP[:, 1, :], ln2_b[L:L + 1, :].broadcast_to([128, D]), 'lnp', writes=['LNP'])
                    for t in range(NB):
                        layer_norm_tile(ACC[:, t, :], ('ACC', t), LNP, ACC[:, t, :], ('ACC', t), (STt, MVt, RSt))
                        T.dma('sp', xout_d[t * 128:(t + 1) * 128, :], ACC[:, t, :], 'xo%d' % (t % 4), reads=[('ACC', t)], writes=[('xo', t)])
                    T.barrier()

        def chk(name):
            if stop == name and not T.dead:
                T.barrier()
                T.final_wait('sp')
                T.dead = True

        try:
            chk('prologue')
            for seq in range(nseq):
                tok0 = seq * S
                for idx, li in enumerate(layers):
                    xin_d = x_in[tok0:tok0 + S, :] if idx == 0 else xs_d
                    xout_d = y_out[tok0:tok0 + S, :] if idx == len(layers) - 1 else xs_d
                    mixer(seq, li, xin_d)
                    chk('mixer')
                    ffn(seq, li, xout_d)
        except StopBuild:
            T.barrier()
        T.final_wait('sp')
        build.n_ins = T.n_ins
    return nc


_CACHE = {}


def host_inputs(inputs, nseq_total, ncores):
    consts = make_consts()
    rb = np.asarray(inputs['rel_bias'], dtype=np.float32)
    tabs = bias_index_tables()
    heads = np.array([head_of(g, i) for g in range(4) for i in range(4)])
    biasg = np.stack([rb[tabs[k]][:, :, heads].transpose(0, 2, 1).reshape(128, 16 * 128) for k in range(2)]).astype(np.float32)
    biasc = np.broadcast_to(rb[15, heads][None, :, None], (128, 16, 128)).reshape(128, 16 * 128).astype(np.float32)
    shared = {k: np.ascontiguousarray(np.asarray(v, dtype=np.float32)) for k, v in inputs.items() if k not in ('x', 'p', 'rel_bias')}
    shared.update(c_ident=consts['ident'], c_i4=consts['i4'], c_admneg=consts['admneg'], c_adm01=consts['adm01'],
                  c_biasg=np.ascontiguousarray(biasg), c_biasc=np.ascontiguousarray(biasc))
    x = np.asarray(inputs['x'], dtype=np.float32)
    p = np.asarray(inputs['p'], dtype=np.float32)
    per = nseq_total // ncores
    in_maps = []
    for c in range(ncores):
        m = dict(shared)
        m['x'] = np.ascontiguousarray(x[c * per:(c + 1) * per].reshape(per * S, D))
        m['p'] = np.ascontiguousarray(p[:, c * per:(c + 1) * per].reshape(DEPTH, per * S, PLE))
        in_maps.append(m)
    return in_maps


def kernel(**inputs):
    B = inputs['x'].shape[0]
    per = B // N_CORES
    key = ('full', per)
    if key not in _CACHE:
        _CACHE[key] = build(per, list(range(DEPTH)))
    nc = _CACHE[key]
    in_maps = host_inputs(inputs, B, N_CORES)
    res = run_bass_kernel_spmd(nc, in_maps, core_ids=list(range(N_CORES)))
    out = np.concatenate([np.asarray(r['y']).reshape(per, S, D) for r in res.results], axis=0)
    return out.astype(np.float32)
```

```python
import numpy as np
from contextlib import ExitStack
import concourse.bass as bass
import concourse.mybir as mybir
from concourse.bass_utils import run_bass_kernel_spmd

F32 = mybir.dt.float32
BF16 = mybir.dt.bfloat16
AF = mybir.ActivationFunctionType
ALU = mybir.AluOpType

S = 2048
D = 1024
NB = 16
NKC = 8
DEPTH = 4
D_FF = 2816
D_FFE = 3584
NEXP = 8
PLE = 256
DIN = 5832
OFF = dict(q=0, k=1024, v=1088, qi=1152, ki=1664, wi=1728, xr=1736, yr=2760, ga=3784, gr=4808)
ALPHA = float((2 * DEPTH) ** 0.25)
EPS = 1e-5
NEG = -1.0e30
MASKMAG = 30000.0
NBIS = 22
N_CORES = 8


class Trk:
    def __init__(self, nc, stack):
        self.nc = nc
        self.stack = stack
        self.engs = {'pe': nc.tensor, 'act': nc.scalar, 'dve': nc.vector, 'pool': nc.gpsimd, 'sp': nc.sync}
        self.esem = {}
        self.ecnt = {}
        self.nsem = 0
        for e in self.engs:
            self.esem[e] = self._newsem(e)
            self.ecnt[e] = 0
        self.waited = {}
        self.lastw = {}
        self.readers = {}
        self.dsem = {}
        self.dcnt = {}
        self.n_ins = 0
        self.dead = False

    def _newsem(self, tag):
        self.nsem += 1
        return self.stack.enter_context(self.nc.semaphore("s%d_%s" % (self.nsem, tag)))

    def _wait(self, eng, ev):
        sem, val, src = ev
        if src.startswith('dma:') and self.dsem.get(src[4:]) is sem:
            val = max(val, self.dcnt[src[4:]])
        k = (eng, sem.name if hasattr(sem, 'name') else id(sem))
        if self.waited.get(k, 0) >= val:
            return
        self.engs[eng].wait_ge(sem, val)
        self.waited[k] = val

    def _deps(self, eng, reads, writes):
        deps = []
        for r in reads:
            ev = self.lastw.get(r)
            if ev is not None:
                deps.append(ev)
        for w in writes:
            ev = self.lastw.get(w)
            if ev is not None:
                deps.append(ev)
            for ev in self.readers.get(w, {}).values():
                deps.append(ev)
        for ev in deps:
            if ev[2] == eng:
                if eng == 'pe':
                    continue
                if ev[0] is self.esem[eng] and self.ecnt[eng] - ev[1] >= 3:
                    continue
            self._wait(eng, ev)

    def _record(self, ev, key, reads, writes):
        for r in reads:
            self.readers.setdefault(r, {})[key] = ev
        for w in writes:
            self.lastw[w] = ev
            self.readers[w] = {}

    def op(self, eng, fn, reads=(), writes=()):
        if self.dead:
            return None
        self._deps(eng, reads, writes)
        ins = fn(self.engs[eng])
        if self.ecnt[eng] >= 60000:
            self.esem[eng] = self._newsem(eng)
            self.ecnt[eng] = 0
        self.ecnt[eng] += 1
        ins.then_inc(self.esem[eng], 1)
        self.n_ins += 1
        ev = (self.esem[eng], self.ecnt[eng], eng)
        self._record(ev, eng, reads, writes)
        return ev

    def dma(self, q, out, in_, key, reads=(), writes=(), slow=False):
        if self.dead:
            return None
        self._deps(q, reads, writes)
        if key not in self.dsem:
            self.dsem[key] = self._newsem("d")
            self.dcnt[key] = 0
        if self.dcnt[key] >= 60000 * 16:
            self.dsem[key] = self._newsem("d")
            self.dcnt[key] = 0
        if self.dcnt[key] > 0:
            self._wait(q, (self.dsem[key], self.dcnt[key], 'dma:' + key))
        if slow:
            ins = self.engs[q].dma_start(out=out, in_=in_, allow_slow_non_contiguous=True)
        else:
            ins = self.engs[q].dma_start(out=out, in_=in_)
        self.dcnt[key] += 16
        ins.then_inc(self.dsem[key], 16)
        self.n_ins += 1
        ev = (self.dsem[key], self.dcnt[key], 'dma:' + key)
        self._record(ev, 'dma:' + key, reads, writes)
        return ev

    def barrier(self, engines=None):
        if self.dead:
            return
        evs = []
        for e in self.engs:
            if self.ecnt[e] > 0:
                evs.append((self.esem[e], self.ecnt[e], e))
        for k in self.dsem:
            if self.dcnt[k] > 0:
                evs.append((self.dsem[k], self.dcnt[k], 'dma:' + k))
        for e in self.engs:
            for ev in evs:
                if ev[2] == e:
                    continue
                self._wait(e, ev)
        self.lastw = {}
        self.readers = {}

    def final_wait(self, eng='sp'):
        if self.dead:
            return
        for k in self.dsem:
            if self.dcnt[k] > 0:
                self._wait(eng, (self.dsem[k], self.dcnt[k], 'dma:' + k))
        for e in self.engs:
            if e != eng and self.ecnt[e] > 0:
                self._wait(eng, (self.esem[e], self.ecnt[e], e))


def t5_bucket_np(rel):
    half = 16
    max_exact = 8
    ret = np.where(rel > 0, half, 0)
    n = np.abs(rel)
    nf = np.maximum(n, 1).astype(np.float32)
    large = max_exact + (np.log(nf / np.float32(max_exact)) / np.float32(np.log(128 / max_exact)) * (half - max_exact)).astype(np.int32)
    large = np.minimum(large, half - 1)
    return ret + np.where(n < max_exact, n, large)


def head_of(g, i):
    return 2 * ((g // 2) * 4 + i) + (g % 2)


def make_consts():
    c = {}
    c['ident'] = np.eye(128, dtype=np.float32)
    c['i4'] = np.tile(np.eye(128, dtype=np.float32) * MASKMAG, (1, 4))
    q = np.arange(128)[:, None]
    s = np.arange(128)[None, :]
    adm = (s < 64) | (q >= 64)
    c['admneg'] = np.where(adm, 0.0, NEG).astype(np.float32)
    c['adm01'] = np.where(adm, 0.0, -1.0).astype(np.float32)
    return c


def bias_index_tables():
    sl = np.arange(128)[:, None]
    ql = np.arange(128)[None, :]
    out = []
    for kind in range(2):
        rel = sl - ql - 128 * kind
        out.append(t5_bucket_np(rel))
    return out


class StopBuild(Exception):
    pass


def build(nseq, layers, dbg=None, stop=None):
    nc = bass.Bass("TRN2", target_bir_lowering=False)
    NT = nseq * S
    dr = {}

    def din(name, shape, dt=F32):
        dr[name] = nc.dram_tensor(name, list(shape), dt, kind="ExternalInput").ap()
        return dr[name]

    x_in = din('x', [NT, D])
    p_in = din('p', [DEPTH, NT, PLE])
    w_in = din('w_in', [DEPTH, D, DIN])
    conv_w = din('conv_w', [DEPTH, 4, D])
    conv_b = din('conv_b', [DEPTH, D])
    lru_wa = din('lru_wa', [DEPTH, 8, 128, 128])
    lru_ba = din('lru_ba', [DEPTH, D])
    lru_wx = din('lru_wx', [DEPTH, 8, 128, 128])
    lru_bx = din('lru_bx', [DEPTH, D])
    lru_lam = din('lru_lam', [DEPTH, D])
    w_br_attn = din('w_br_attn', [DEPTH, D, D])
    w_br_rnn = din('w_br_rnn', [DEPTH, D, D])
    w_o = din('w_o', [DEPTH, D, D])
    ln1_g = din('ln1_g', [DEPTH, D])
    ln1_b = din('ln1_b', [DEPTH, D])
    ffn_w_gate = din('ffn_w_gate', [2, D, D_FF])
    ffn_w_up = din('ffn_w_up', [2, D, D_FF])
    ffn_w_down = din('ffn_w_down', [2, D_FF, D])
    moe_router = din('moe_router', [2, D, NEXP])
    moe_router_b = din('moe_router_b', [2, NEXP])
    moe_w_gate = din('moe_w_gate', [2, NEXP, D, D_FFE])
    moe_w_up = din('moe_w_up', [2, NEXP, D, D_FFE])
    moe_w_down = din('moe_w_down', [2, NEXP, D_FFE, D])
    ple_w_gate = din('ple_w_gate', [DEPTH, D, D])
    ple_w_proj = din('ple_w_proj', [DEPTH, PLE, D])
    ln2_g = din('ln2_g', [DEPTH, D])
    ln2_b = din('ln2_b', [DEPTH, D])
    c_ident = din('c_ident', [128, 128])
    c_i4 = din('c_i4', [128, 512])
    c_admneg = din('c_admneg', [128, 128])
    c_adm01 = din('c_adm01', [128, 128])
    c_biasg = din('c_biasg', [2, 128, 16 * 128])
    c_biasc = din('c_biasc', [128, 16 * 128])
    y_out = nc.dram_tensor('y', [NT, D], F32, kind="ExternalOutput").ap()
    xs_d = nc.dram_tensor('xs', [S, D], F32, kind="Internal").ap()
    x1s_d = nc.dram_tensor('x1s', [S, D], F32, kind="Internal").ap()
    dbg_out = {}
    if dbg:
        for name, shape in dbg.items():
            dbg_out[name] = nc.dram_tensor('dbg_' + name, list(shape), F32, kind="ExternalOutput").ap()

    with ExitStack() as top:
        T = Trk(nc, top)

        sbn = [0]

        def sb(stack, name, shape, dt):
            sbn[0] += 1
            return stack.enter_context(nc.sbuf_tensor("%s_%d" % (name, sbn[0]), list(shape), dt))

        ps = top.enter_context(nc.psum_tensor("ps", [128, 8, 512], F32))
        bank_list = [list(range(8))]
        bank_ctr = [0]

        def next_bank():
            bl = bank_list[0]
            b = bl[bank_ctr[0] % len(bl)]
            bank_ctr[0] += 1
            return b

        evac_ctr = [0]

        def evac_eng():
            evac_ctr[0] += 1
            return 'act' if evac_ctr[0] % 2 == 0 else 'dve'

        def copy_op(eng, out, in_, reads, writes):
            if eng == 'act':
                return T.op('act', lambda e: e.activation(out=out, in_=in_, func=AF.Copy), reads, writes)
            return T.op(eng, lambda e: e.tensor_copy(out, in_), reads, writes)

        IDENT = sb(top, "IDENT", [128, 128], BF16)
        I4 = sb(top, "I4", [128, 512], BF16)
        ADMNEG = sb(top, "ADMNEG", [128, 128], F32)
        ADM01 = sb(top, "ADM01", [128, 128], BF16)
        BIAS = sb(top, "BIAS", [128, 2, 2048], BF16)
        XT = sb(top, "XT", [128, NKC, S], BF16)
        CW = sb(top, "CW", [128, 4, 8], F32)
        CB = sb(top, "CB", [128, 8], F32)
        BA = sb(top, "BA", [128, 8], F32)
        BX = sb(top, "BX", [128, 8], F32)
        LAM = sb(top, "LAM", [128, 8], F32)
        CL = sb(top, "CL", [128, 8], F32)
        CL2 = sb(top, "CL2", [128, 8], F32)
        RB = sb(top, "RB", [128, 8], F32)
        EPSB = sb(top, "EPSB", [128, 1], F32)
        T.op('pool', lambda e: e.memset(EPSB[:], EPS), writes=['EPSB'])

        T.dma('pool', IDENT[:], c_ident, 'c0', writes=['IDENT'])
        T.dma('pool', I4[:], c_i4, 'c0', writes=['I4'])
        T.dma('pool', ADM01[:], c_adm01, 'c0', writes=['ADM01'])
        T.dma('sp', ADMNEG[:], c_admneg, 'c1', writes=['ADMNEG'])
        with ExitStack() as st:
            BG = sb(st, "BG", [128, 2, 2048], F32)
            BC = sb(st, "BC", [128, 2048], F32)
            T.dma('sp', BG[:, 0, :], c_biasg[0], 'c1', writes=['BG'])
            T.dma('sp', BG[:, 1, :], c_biasg[1], 'c1', writes=['BG'])
            T.dma('sp', BC[:], c_biasc, 'c1', writes=['BC'])
            for k in range(2):
                T.op('dve', lambda e, k=k: e.tensor_tensor(out=BG[:, k, :], in0=BG[:, k, :], in1=BC[:], op=ALU.subtract),
                     reads=['BC', 'BG'], writes=['BG'])
                T.op('dve', lambda e, k=k: e.tensor_scalar(out=BIAS[:, k, :], in0=BG[:, k, :], scalar1=8.0, scalar2=None, op0=ALU.mult),
                     reads=['BG'], writes=['BIAS'])
            T.barrier()

        def load_w(dst, src2d, key, wregion):
            T.dma('pool', dst, src2d.rearrange("(kc p) n -> p kc n", p=128), key, writes=[wregion])

        def build_xt(stack_parent, dstT, src_tok, nkc, tag):
            with ExitStack() as st:
                XB = sb(st, "XB" + tag, [128, 2, 4, nkc * 128], BF16)
                for tg in range(4):
                    sl = tg % 2
                    T.dma('pool', XB[:, sl, :, :],
                          src_tok[tg * 512:(tg + 1) * 512, :].rearrange("(t p) f -> p t f", p=128),
                          'xb%d' % sl, writes=[('XB', sl)])
                    for kc in range(nkc):
                        b = next_bank()
                        for t in range(4):
                            T.op('pe', lambda e, b=b, t=t, kc=kc, sl=sl: e.matmul(
                                ps[:, b, t * 128:(t + 1) * 128], lhsT=XB[:, sl, t, kc * 128:(kc + 1) * 128], rhs=IDENT[:],
                                start=True, stop=True), reads=[('XB', sl), 'IDENT'], writes=[('ps', b)])
                        copy_op(evac_eng(), dstT[:, kc, tg * 512:(tg + 1) * 512], ps[:, b, :],
                                reads=[('ps', b)], writes=[(tag, kc, tg)])
                T.barrier()

        def proj_fm(WTt, col0, rhsT, nkc, tg, b):
            for kc in range(nkc):
                T.op('pe', lambda e, kc=kc: e.matmul(ps[:, b, :], lhsT=WTt[:, kc, col0:col0 + 128],
                                                     rhs=rhsT[:, kc, tg * 512:(tg + 1) * 512],
                                                     start=(kc == 0), stop=(kc == nkc - 1)),
                     reads=['WT_any', 'RHS_any'], writes=[('ps', b)])

        def layer_norm_tile(Yt, ykey, LNP, out_ap, okey, st_tiles):
            STt, MVt, RSTDt = st_tiles
            for h in range(2):
                T.op('dve', lambda e, h=h: e.bn_stats(out=STt[:, h, :], in_=Yt[:, h * 512:(h + 1) * 512]),
                     reads=[ykey], writes=['LNST'])
            T.op('dve', lambda e: e.bn_aggr(out=MVt[:], in_=STt[:].rearrange("p a b -> p (a b)")), reads=['LNST'], writes=['LNMV'])
            T.op('act', lambda e: e.activation(out=RSTDt[:], in_=MVt[:, 1:2], func=AF.Sqrt, bias=EPSB[:, 0:1], scale=1.0),
                 reads=['LNMV'], writes=['LNRS'])
            T.op('dve', lambda e: e.reciprocal(RSTDt[:], RSTDt[:]), reads=['LNRS'], writes=['LNRS'])
            T.op('dve', lambda e: e.tensor_scalar(out=Yt[:], in0=Yt[:], scalar1=MVt[:, 0:1], scalar2=RSTDt[:, 0:1],
                                                  op0=ALU.subtract, op1=ALU.mult), reads=[ykey, 'LNMV', 'LNRS'], writes=[ykey])
            T.op('pool', lambda e: e.tensor_tensor(out=Yt[:], in0=Yt[:], in1=LNP[:, 0, :], op=ALU.mult), reads=[ykey, 'LNP'], writes=[ykey])
            T.op('pool', lambda e: e.tensor_tensor(out=out_ap, in0=Yt[:], in1=LNP[:, 1, :], op=ALU.add), reads=[ykey, 'LNP'], writes=[okey])

        def mixer(seq, li, xin_d):
            L = li
            tok0 = seq * S
            for j in range(4):
                T.dma('sp', CW[:, j, :], conv_w[L, j].rearrange("(n p) -> p n", p=128), 'prm', writes=['PRM'], slow=True)
            T.dma('sp', CB[:], conv_b[L].rearrange("(n p) -> p n", p=128), 'prm', writes=['PRM'], slow=True)
            T.dma('sp', BA[:], lru_ba[L].rearrange("(n p) -> p n", p=128), 'prm', writes=['PRM'], slow=True)
            T.dma('sp', BX[:], lru_bx[L].rearrange("(n p) -> p n", p=128), 'prm', writes=['PRM'], slow=True)
            T.dma('sp', LAM[:], lru_lam[L].rearrange("(n p) -> p n", p=128), 'prm', writes=['PRM'], slow=True)
            T.op('act', lambda e: e.activation(out=CL[:], in_=LAM[:], func=AF.Exp, scale=-1.0), reads=['PRM'], writes=['CL'])
            T.op('act', lambda e: e.activation(out=CL[:], in_=CL[:], func=AF.Ln, bias=1.0), reads=['CL'], writes=['CL'])
            T.op('dve', lambda e: e.tensor_scalar(out=CL2[:], in0=CL[:], scalar1=-16.0, scalar2=None, op0=ALU.mult), reads=['CL'], writes=['CL2'])
            T.op('dve', lambda e: e.tensor_scalar(out=CL[:], in0=CL[:], scalar1=-8.0, scalar2=None, op0=ALU.mult), reads=['CL', 'CL2'], writes=['CL'])

            chk('params')
            build_xt(None, XT, xin_d, NKC, 'XT')
            chk('xt')

            with ExitStack() as mst:
                QT = sb(mst, "QT", [128, NKC, S], BF16)
                with ExitStack() as ast:
                    WT = sb(ast, "WTa", [128, 2, NKC, 512], BF16)
                    KTE = sb(ast, "KTE", [128, S], BF16)
                    KTO = sb(ast, "KTO", [128, S], BF16)
                    KITE = sb(ast, "KITE", [128, S], BF16)
                    KITO = sb(ast, "KITO", [128, S], BF16)
                    QIT = sb(ast, "QIT", [128, 4, S], BF16)
                    V1E = sb(ast, "V1E", [128, NB, 128], BF16)
                    V1O = sb(ast, "V1O", [128, NB, 128], BF16)
                    WI = sb(ast, "WI", [128, NB, 8], F32)
                    SC = sb(ast, "SC", [128, S], F32)
                    BIS = sb(ast, "BIS", [128, 8], F32)
                    RNGS = sb(ast, "RNGS", [128, 32], F32)
                    P2ROW = sb(ast, "P2ROW", [128, 32], F32)
                    for r in range(1, NBIS + 1):
                        T.op('pool', lambda e, r=r: e.memset(P2ROW[:, r - 1:r], float(2.0 ** -r)), writes=['P2ROW'])
                    RELU = sb(ast, "RELU", [128, 2, 512], F32)
                    MASK = sb(ast, "MASK", [128, 2, S], BF16)
                    PT = sb(ast, "PT", [128, 2, 2048], BF16)
                    RS = sb(ast, "RS", [128, 2, 512], F32)

                    T.op('pool', lambda e: e.memset(KTE[:], 0.0), writes=['KTE'])
                    T.op('pool', lambda e: e.memset(KTO[:], 0.0), writes=['KTO'])
                    T.op('pool', lambda e: e.memset(KITE[:], 0.0), writes=['KITE'])
                    T.op('pool', lambda e: e.memset(KITO[:], 0.0), writes=['KITO'])
                    T.op('pool', lambda e: e.memset(V1E[:], 1.0), writes=['V1E'])
                    T.op('pool', lambda e: e.memset(V1O[:], 1.0), writes=['V1O'])

                    wslot = [0]

                    def wt_load(cols, width):
                        s_ = wslot[0] % 2
                        wslot[0] += 1
                        load_w(WT[:, s_, :, 0:width], w_in[L][:, cols:cols + width], 'wa%d' % s_, ('WT', s_))
                        return s_

                    def proj(s_, col0, tg, b):
                        for kc in range(NKC):
                            T.op('pe', lambda e, kc=kc: e.matmul(ps[:, b, :], lhsT=WT[:, s_, kc, col0:col0 + 128],
                                                                 rhs=XT[:, kc, tg * 512:(tg + 1) * 512],
                                                                 start=(kc == 0), stop=(kc == NKC - 1)),
                                 reads=[('WT', s_)], writes=[('ps', b)])

                    for hf in range(2):
                        s_ = wt_load(OFF['q'] + hf * 512, 512)
                        for cl in range(4):
                            c = hf * 4 + cl
                            for tg in range(4):
                                b = next_bank()
                                proj(s_, cl * 128, tg, b)
                                copy_op(evac_eng(), QT[:, c, tg * 512:(tg + 1) * 512], ps[:, b, :],
                                        reads=[('ps', b)], writes=[('QTc', c, tg)])
                    chk('projq')
                    s_ = wt_load(OFF['qi'], 512)
                    for c in range(4):
                        for tg in range(4):
                            b = next_bank()
                            proj(s_, c * 128, tg, b)
                            copy_op(evac_eng(), QIT[:, c, tg * 512:(tg + 1) * 512], ps[:, b, :],
                                    reads=[('ps', b)], writes=[('QIT', c, tg)])
                    chk('projqi')
                    s_ = wslot[0] % 2
                    wslot[0] += 1
                    wl = w_in[L]
                    for (dst0, src0, wd) in ((0, OFF['k'], 64), (64, OFF['k'], 64), (128, OFF['ki'], 64), (192, OFF['ki'], 64),
                                             (256, OFF['v'], 64), (320, OFF['wi'], 8)):
                        load_w(WT[:, s_, :, dst0:dst0 + wd], wl[:, src0:src0 + wd], 'wa%d' % s_, ('WT', s_))
                    for tg in range(4):
                        b = next_bank()
                        proj(s_, 0, tg, b)
                        copy_op('act', KTE[0:64, tg * 512:(tg + 1) * 512], ps[0:64, b, :], reads=[('ps', b)], writes=['KTE'])
                        copy_op('act', KTO[64:128, tg * 512:(tg + 1) * 512], ps[64:128, b, :], reads=[('ps', b)], writes=['KTO', ('ps', b)])
                        b = next_bank()
                        proj(s_, 128, tg, b)
                        copy_op('act', KITE[0:64, tg * 512:(tg + 1) * 512], ps[0:64, b, :], reads=[('ps', b)], writes=['KITE'])
                        copy_op('act', KITO[64:128, tg * 512:(tg + 1) * 512], ps[64:128, b, :], reads=[('ps', b)], writes=['KITO', ('ps', b)])
                    chk('projk')
                    for t4 in range(4):
                        b = next_bank()
                        for tl in range(4):
                            t = t4 * 4 + tl
                            for kc in range(NKC):
                                T.op('pe', lambda e, kc=kc, t=t, tl=tl: e.matmul(
                                    ps[:, b, tl * 128:tl * 128 + 72], lhsT=XT[:, kc, t * 128:(t + 1) * 128],
                                    rhs=WT[:, s_, kc, 256:328], start=(kc == 0), stop=(kc == NKC - 1)),
                                     reads=[('WT', s_)], writes=[('ps', b)])
                        pv = ps[:, b, :].rearrange("p (a c) -> p a c", a=4)
                        copy_op('act', V1E[:, t4 * 4:(t4 + 1) * 4, 0:64], pv[:, :, 0:64], reads=[('ps', b)], writes=['V1E'])
                        copy_op('act', WI[:, t4 * 4:(t4 + 1) * 4, :], pv[:, :, 64:72], reads=[('ps', b)], writes=['WI', ('ps', b)])
                        copy_op('pool', V1O[:, t4 * 4:(t4 + 1) * 4, 64:128], V1E[:, t4 * 4:(t4 + 1) * 4, 0:64], reads=['V1E'], writes=['V1O'])
                    T.barrier()

                    chk('proj')

                    bank_list[0] = [0, 1, 2, 3]

                    def score(j):
                        nk = 128 * (j + 1)
                        ms = j % 2
                        if j < 2:
                            return
                        nsl = (nk + 511) // 512
                        for h in range(8):
                            c, par = h // 2, h % 2
                            KI = KITE if par == 0 else KITO
                            for si in range(nsl):
                                k0 = si * 512
                                k1 = min(nk, k0 + 512)
                                b = next_bank()
                                T.op('pe', lambda e, b=b, c=c, KI=KI, k0=k0, k1=k1: e.matmul(
                                    ps[:, b, 0:k1 - k0], lhsT=QIT[:, c, j * 128:(j + 1) * 128], rhs=KI[:, k0:k1],
                                    start=True, stop=True), reads=[], writes=[('ps', b)])
                                rs_ = (h * nsl + si) % 2
                                T.op('act', lambda e, b=b, rs_=rs_, k0=k0, k1=k1: e.activation(
                                    out=RELU[:, rs_, 0:k1 - k0], in_=ps[:, b, 0:k1 - k0], func=AF.Relu),
                                     reads=[('ps', b)], writes=[('RELU', rs_)])
                                if h == 0:
                                    T.op('dve', lambda e, rs_=rs_, k0=k0, k1=k1, h=h: e.tensor_scalar(
                                        out=SC[:, k0:k1], in0=RELU[:, rs_, 0:k1 - k0], scalar1=WI[:, j, h:h + 1], scalar2=None,
                                        op0=ALU.mult), reads=[('RELU', rs_)], writes=[('SC', si)])
                                else:
                                    T.op('dve', lambda e, rs_=rs_, k0=k0, k1=k1, h=h: e.scalar_tensor_tensor(
                                        out=SC[:, k0:k1], in0=RELU[:, rs_, 0:k1 - k0], scalar=WI[:, j, h:h + 1], in1=SC[:, k0:k1],
                                        op0=ALU.mult, op1=ALU.add), reads=[('RELU', rs_), ('SC', si)], writes=[('SC', si)])
                        allsc = [('SC', si) for si in range(nsl)]
                        T.op('dve', lambda e: e.tensor_reduce(out=BIS[:, 0:1], in_=SC[:, 0:nk], axis=mybir.AxisListType.X, op=ALU.min),
                             reads=allsc, writes=['BLO'])
                        T.op('dve', lambda e: e.tensor_reduce(out=BIS[:, 1:2], in_=SC[:, 0:nk], axis=mybir.AxisListType.X, op=ALU.max),
                             reads=allsc, writes=['BRG'])
                        T.op('dve', lambda e: e.tensor_tensor(out=BIS[:, 1:2], in0=BIS[:, 1:2], in1=BIS[:, 0:1], op=ALU.subtract),
                             reads=['BLO', 'BRG'], writes=['BRG'])
                        T.op('dve', lambda e: e.tensor_tensor(out=SC[:, j * 128:nk], in0=SC[:, j * 128:nk], in1=ADMNEG[:], op=ALU.add),
                             reads=allsc, writes=allsc)
                        T.op('dve', lambda e: e.tensor_scalar(out=RNGS[:, 0:NBIS], in0=P2ROW[:, 0:NBIS], scalar1=BIS[:, 1:2], scalar2=None, op0=ALU.mult),
                             reads=['BRG'], writes=['RNGS'])
                        T.op('dve', lambda e: e.tensor_tensor(out=BIS[:, 2:3], in0=BIS[:, 0:1], in1=RNGS[:, 0:1], op=ALU.add),
                             reads=['BLO', 'RNGS'], writes=['BMID'])
                        for r in range(1, NBIS + 1):
                            T.op('dve', lambda e: e.tensor_scalar(out=MASK[:, ms, 0:nk], in0=SC[:, 0:nk], scalar1=BIS[:, 2:3], scalar2=-255.5,
                                                                  op0=ALU.is_ge, op1=ALU.add, accum_out=BIS[:, 3:4]),
                                 reads=allsc + ['BMID'], writes=[('MASK', ms), 'BCNT'])
                            if r < NBIS:
                                T.op('dve', lambda e: e.tensor_scalar(out=BIS[:, 4:5], in0=BIS[:, 3:4], scalar1=0.0, scalar2=-0.5,
                                                                      op0=ALU.is_ge, op1=ALU.add), reads=['BCNT'], writes=['BTT'])
                                T.op('dve', lambda e, r=r: e.scalar_tensor_tensor(out=BIS[:, 2:3], in0=BIS[:, 4:5], scalar=RNGS[:, r - 1:r], in1=BIS[:, 2:3],
                                                                                 op0=ALU.mult, op1=ALU.add), reads=['BTT', 'RNGS', 'BMID'], writes=['BMID'])
                            else:
                                T.op('dve', lambda e: e.tensor_scalar(out=BIS[:, 4:5], in0=BIS[:, 3:4], scalar1=0.0, scalar2=-1.0,
                                                                      op0=ALU.is_ge, op1=ALU.add), reads=['BCNT'], writes=['BTT'])
                                T.op('dve', lambda e, r=r: e.scalar_tensor_tensor(out=BIS[:, 0:1], in0=BIS[:, 4:5], scalar=RNGS[:, r - 1:r], in1=BIS[:, 2:3],
                                                                                 op0=ALU.mult, op1=ALU.add), reads=['BTT', 'RNGS', 'BMID'], writes=['BLO'])
                        T.op('dve', lambda e: e.tensor_scalar(out=MASK[:, ms, 0:nk], in0=SC[:, 0:nk], scalar1=BIS[:, 0:1], scalar2=-1.0,
                                                              op0=ALU.is_ge, op1=ALU.add), reads=allsc + ['BLO'], writes=[('MASK', ms)])

                    pt_ctr = [0]

                    def attend(j):
                        ms = j % 2
                        q0 = j * 128
                        for kb in range(j + 1):
                            near = j - kb
                            pslot = pt_ctr[0] % 2
                            pt_ctr[0] += 1
                            for g in range(4):
                                c0 = (g // 2) * 4
                                KT = KTE if g % 2 == 0 else KTO
                                V1 = V1E if g % 2 == 0 else V1O
                                b = next_bank()
                                has_mask = (j >= 2) or (kb == j)
                                has_bias = near <= 1
                                T.op('pe', lambda e, b=b, KT=KT, c0=c0: e.matmul(
                                    ps[:, b, :].rearrange("p (a c) -> p a c", a=4), lhsT=KT[:, kb * 128:(kb + 1) * 128],
                                    rhs=QT[:, c0:c0 + 4, q0:q0 + 128], start=True, stop=not (has_mask or has_bias)),
                                     reads=[('QTb', j)], writes=[('ps', b)])
                                if has_mask:
                                    if j >= 2:
                                        ml = MASK[:, ms, kb * 128:(kb + 1) * 128]
                                        rd = [('MASK', ms)]
                                    else:
                                        ml = ADM01[:]
                                        rd = []
                                    T.op('pe', lambda e, b=b, ml=ml: e.matmul(ps[:, b, :], lhsT=ml, rhs=I4[:], start=False, stop=not has_bias),
                                         reads=rd, writes=[('ps', b)])
                                if has_bias:
                                    T.op('pe', lambda e, b=b, g=g: e.matmul(ps[:, b, :], lhsT=IDENT[:], rhs=BIAS[:, near, g * 512:(g + 1) * 512],
                                                                            start=False, stop=True), reads=[], writes=[('ps', b)])
                                T.op('act', lambda e, b=b, g=g: e.activation(out=PT[:, pslot, g * 512:(g + 1) * 512], in_=ps[:, b, :],
                                                                             func=AF.Exp, scale=0.125),
                                     reads=[('ps', b)], writes=[('PT', pslot, g)])
                                T.op('pe', lambda e, g=g, V1=V1: e.matmul(ps[:, 4 + g, :], lhsT=V1[:, kb, :], rhs=PT[:, pslot, g * 512:(g + 1) * 512],
                                                                          start=(kb == 0), stop=(kb == j)),
                                     reads=[('PT', pslot, g)], writes=[('oacc', g)])
                        for g in range(4):
                            c0 = (g // 2) * 4
                            rsl = g % 2
                            if g % 2 == 0:
                                T.op('act', lambda e, g=g: e.activation(out=RS[0:64, rsl, :], in_=ps[64:128, 4 + g, :], func=AF.Ln),
                                     reads=[('oacc', g)], writes=[('RS', rsl)])
                                T.op('act', lambda e, g=g: e.activation(out=RS[0:64, rsl, :], in_=RS[0:64, rsl, :], func=AF.Exp, scale=-1.0),
                                     reads=[('RS', rsl)], writes=[('RS', rsl)])
                                T.op('dve', lambda e, g=g: e.tensor_tensor(
                                    out=QT[0:64, c0:c0 + 4, q0:q0 + 128], in0=ps[0:64, 4 + g, :].rearrange("p (a c) -> p a c", a=4),
                                    in1=RS[0:64, rsl, :].rearrange("p (a c) -> p a c", a=4), op=ALU.mult),
                                     reads=[('oacc', g), ('RS', rsl)], writes=[('oacc', g), ('QTb', j)])
                            else:
                                T.op('act', lambda e, g=g: e.activation(out=RS[64:128, rsl, :], in_=ps[0:64, 4 + g, :], func=AF.Ln),
                                     reads=[('oacc', g)], writes=[('RS', rsl)])
                                T.op('act', lambda e, g=g: e.activation(out=RS[64:128, rsl, :], in_=RS[64:128, rsl, :], func=AF.Exp, scale=-1.0),
                                     reads=[('RS', rsl)], writes=[('RS', rsl)])
                                T.op('dve', lambda e, g=g: e.tensor_tensor(
                                    out=QT[64:128, c0:c0 + 4, q0:q0 + 128], in0=ps[64:128, 4 + g, :].rearrange("p (a c) -> p a c", a=4),
                                    in1=RS[64:128, rsl, :].rearrange("p (a c) -> p a c", a=4), op=ALU.mult),
                                     reads=[('oacc', g), ('RS', rsl)], writes=[('oacc', g), ('QTb', j)])

                    score(0)
                    for j in range(NB):
                        if j + 1 < NB:
                            score(j + 1)
                        attend(j)
                    T.barrier()
                    bank_list[0] = list(range(8))
                if dbg and 'oT' in dbg:
                    with ExitStack() as dst_:
                        DT_ = sb(dst_, "DBGT", [128, S], F32)
                        for c in range(NKC):
                            T.op('dve', lambda e, c=c: e.tensor_copy(DT_[:], QT[:, c, :]), reads=['dbgd'], writes=['dbgt'])
                            T.dma('sp', dbg_out['oT'][c * 128:(c + 1) * 128, :], DT_[:], 'dbg', reads=['dbgt'], writes=['dbgd'])
                        T.barrier()

                MT = sb(mst, "MT", [128, NKC, S], BF16)

                def branch(WBR, gate_off, first):
                    with ExitStack() as bst:
                        WTb = sb(bst, "WTb", [128, 4, NKC, 512], BF16)
                        SGt = sb(bst, "SGt", [128, 2, 512], F32)
                        TMPt = sb(bst, "TMPt", [128, 2, 512], F32)
                        k_ = [0]
                        for hf in range(2):
                            sA = (hf * 2) % 4
                            sG = (hf * 2 + 1) % 4
                            load_w(WTb[:, sA, :, :], WBR[L][:, hf * 512:(hf + 1) * 512], 'wb%d' % sA, ('WTb', sA))
                            load_w(WTb[:, sG, :, :], w_in[L][:, gate_off + hf * 512:gate_off + (hf + 1) * 512], 'wb%d' % sG, ('WTb', sG))
                            for cl in range(4):
                                fo = hf * 4 + cl
                                for tg in range(4):
                                    bA = next_bank()
                                    for kc in range(NKC):
                                        T.op('pe', lambda e, kc=kc: e.matmul(ps[:, bA, :], lhsT=WTb[:, sA, kc, cl * 128:(cl + 1) * 128],
                                                                             rhs=QT[:, kc, tg * 512:(tg + 1) * 512],
                                                                             start=(kc == 0), stop=(kc == NKC - 1)),
                                             reads=[('WTb', sA), 'QTall'], writes=[('ps', bA)])
                                    bG = next_bank()
                                    for kc in range(NKC):
                                        T.op('pe', lambda e, kc=kc: e.matmul(ps[:, bG, :], lhsT=WTb[:, sG, kc, cl * 128:(cl + 1) * 128],
                                                                             rhs=XT[:, kc, tg * 512:(tg + 1) * 512],
                                                                             start=(kc == 0), stop=(kc == NKC - 1)),
                                             reads=[('WTb', sG)], writes=[('ps', bG)])
                                    sl = k_[0] % 2
                                    k_[0] += 1
                                    T.op('act', lambda e: e.activation(out=SGt[:, sl, :], in_=ps[:, bG, :], func=AF.Sigmoid),
                                         reads=[('ps', bG)], writes=[('SG', sl)])
                                    if first:
                                        T.op('dve', lambda e: e.tensor_tensor(out=MT[:, fo, tg * 512:(tg + 1) * 512], in0=ps[:, bA, :],
                                                                              in1=SGt[:, sl, :], op=ALU.mult),
                                             reads=[('ps', bA), ('SG', sl)], writes=[('ps', bA), ('MT', fo, tg)])
                                    else:
                                        T.op('dve', lambda e: e.tensor_tensor(out=TMPt[:, sl, :], in0=ps[:, bA, :], in1=SGt[:, sl, :], op=ALU.mult),
                                             reads=[('ps', bA), ('SG', sl)], writes=[('ps', bA), ('TMP', sl)])
                                        T.op('pool', lambda e: e.tensor_tensor(out=MT[:, fo, tg * 512:(tg + 1) * 512],
                                                                               in0=MT[:, fo, tg * 512:(tg + 1) * 512], in1=TMPt[:, sl, :], op=ALU.add),
                                             reads=[('TMP', sl), ('MT', fo, tg)], writes=[('MT', fo, tg)])
                        T.barrier()

                chk('attn')
                branch(w_br_attn, OFF['ga'], True)
                chk('brA')

                with ExitStack() as rst:
                    WTr = sb(rst, "WTr", [128, 2, NKC, 512], BF16)
                    WGA = sb(rst, "WGA", [128, 8, 128], BF16)
                    WGX = sb(rst, "WGX", [128, 8, 128], BF16)
                    NR = 3
                    XR = sb(rst, "XR", [128, NR, 3 + 512], F32)
                    XC = sb(rst, "XC", [128, NR, 512], F32)
                    XCB = sb(rst, "XCB", [128, NR, 512], BF16)
                    RA = sb(rst, "RA", [128, NR, 512], F32)
                    SQ = sb(rst, "SQ", [128, NR, 512], F32)
                    IU = sb(rst, "IU", [128, NR, 512], F32)
                    HH = sb(rst, "HH", [128, NR, 512], F32)
                    GG = sb(rst, "GG", [128, NR, 512], F32)
                    T.dma('pool', WGA[:], lru_wa[L].rearrange("n c d -> c n d"), 'wg', writes=['WGA'])
                    T.dma('pool', WGX[:], lru_wx[L].rearrange("n c d -> c n d"), 'wg', writes=['WGX'])
                    iters = [(hf, cl, tg) for hf in range(2) for cl in range(4) for tg in range(4)]

                    def stage1(it):
                        hf, cl, tg = iters[it]
                        n = hf * 4 + cl
                        r_ = it % NR
                        rp = (it - 1) % NR
                        K = lambda nm: (nm, r_)
                        tsl = slice(tg * 512, (tg + 1) * 512)
                        if cl == 0 and tg == 0:
                            load_w(WTr[:, 0, :, :], w_in[L][:, OFF['xr'] + hf * 512:OFF['xr'] + (hf + 1) * 512], 'wr0', ('WTr', 0))
                        bX = next_bank()
                        for kc in range(NKC):
                            T.op('pe', lambda e, kc=kc: e.matmul(ps[:, bX, :], lhsT=WTr[:, 0, kc, cl * 128:(cl + 1) * 128], rhs=XT[:, kc, tsl],
                                                                 start=(kc == 0), stop=(kc == NKC - 1)), reads=[('WTr', 0)], writes=[('ps', bX)])
                        if tg == 0:
                            T.op('pool', lambda e: e.memset(XR[:, r_, 0:3], 0.0), writes=[K('XR')])
                        else:
                            T.op('pool', lambda e: e.tensor_copy(XR[:, r_, 0:3], XR[:, rp, 512:515]), reads=[('XR', rp)], writes=[K('XR')])
                        T.op('act', lambda e: e.activation(out=XR[:, r_, 3:515], in_=ps[:, bX, :], func=AF.Copy), reads=[('ps', bX)], writes=[K('XR')])
                        T.op('dve', lambda e: e.tensor_scalar(out=XC[:, r_, :], in0=XR[:, r_, 0:512], scalar1=CW[:, 0, n:n + 1], scalar2=CB[:, n:n + 1],
                                                              op0=ALU.mult, op1=ALU.add), reads=[K('XR'), 'PRM'], writes=[K('XC')])
                        for jt in range(1, 4):
                            T.op('dve', lambda e, jt=jt: e.scalar_tensor_tensor(out=XC[:, r_, :], in0=XR[:, r_, jt:jt + 512], scalar=CW[:, jt, n:n + 1],
                                                                               in1=XC[:, r_, :], op0=ALU.mult, op1=ALU.add),
                                 reads=[K('XR'), K('XC')], writes=[K('XC')])
                        T.op('pool', lambda e: e.tensor_copy(XCB[:, r_, :], XC[:, r_, :]), reads=[K('XC')], writes=[K('XCB')])

                    def stage2(it):
                        hf, cl, tg = iters[it]
                        n = hf * 4 + cl
                        r_ = it % NR
                        rp = (it - 1) % NR
                        K = lambda nm: (nm, r_)
                        tsl = slice(tg * 512, (tg + 1) * 512)
                        if cl == 0 and tg == 0:
                            load_w(WTr[:, 1, :, :], w_in[L][:, OFF['yr'] + hf * 512:OFF['yr'] + (hf + 1) * 512], 'wr1', ('WTr', 1))
                        bR = next_bank()
                        T.op('pe', lambda e: e.matmul(ps[:, bR, :], lhsT=WGA[:, n, :], rhs=XCB[:, r_, :], start=True, stop=True),
                             reads=[K('XCB'), 'WGA'], writes=[('ps', bR)])
                        bI = next_bank()
                        T.op('pe', lambda e: e.matmul(ps[:, bI, :], lhsT=WGX[:, n, :], rhs=XCB[:, r_, :], start=True, stop=True),
                             reads=[K('XCB'), 'WGX'], writes=[('ps', bI)])
                        bY = next_bank()
                        for kc in range(NKC):
                            T.op('pe', lambda e, kc=kc: e.matmul(ps[:, bY, :], lhsT=WTr[:, 1, kc, cl * 128:(cl + 1) * 128], rhs=XT[:, kc, tsl],
                                                                 start=(kc == 0), stop=(kc == NKC - 1)), reads=[('WTr', 1)], writes=[('ps', bY)])
                        T.op('act', lambda e: e.activation(out=RA[:, r_, :], in_=ps[:, bR, :], func=AF.Sigmoid, bias=BA[:, n:n + 1]),
                             reads=[('ps', bR), 'PRM'], writes=[K('RA')])
                        T.op('act', lambda e: e.activation(out=IU[:, r_, :], in_=ps[:, bI, :], func=AF.Sigmoid, bias=BX[:, n:n + 1]),
                             reads=[('ps', bI), 'PRM'], writes=[K('IU')])
                        T.op('act', lambda e: e.activation(out=GG[:, r_, :], in_=ps[:, bY, :], func=AF.Gelu_apprx_tanh),
                             reads=[('ps', bY)], writes=[K('GG')])
                        T.op('act', lambda e: e.activation(out=SQ[:, r_, :], in_=RA[:, r_, :], func=AF.Exp, scale=CL2[:, n:n + 1]),
                             reads=[K('RA'), 'CL2'], writes=[K('SQ')])
                        T.op('act', lambda e: e.activation(out=RA[:, r_, :], in_=RA[:, r_, :], func=AF.Exp, scale=CL[:, n:n + 1]),
                             reads=[K('RA'), 'CL'], writes=[K('RA')])
                        T.op('act', lambda e: e.activation(out=SQ[:, r_, :], in_=SQ[:, r_, :], func=AF.Sqrt, scale=-1.0, bias=1.0),
                             reads=[K('SQ')], writes=[K('SQ')])
                        T.op('pool', lambda e: e.tensor_tensor(out=IU[:, r_, :], in0=IU[:, r_, :], in1=XC[:, r_, :], op=ALU.mult),
                             reads=[K('IU'), K('XC')], writes=[K('IU')])
                        T.op('pool', lambda e: e.tensor_tensor(out=IU[:, r_, :], in0=IU[:, r_, :], in1=SQ[:, r_, :], op=ALU.mult),
                             reads=[K('IU'), K('SQ')], writes=[K('IU')])
                        init = 0.0 if tg == 0 else HH[:, rp, 511:512]
                        T.op('dve', lambda e: e.tensor_tensor_scan(out=HH[:, r_, :], data0=RA[:, r_, :], data1=IU[:, r_, :], initial=init,
                                                                   op0=ALU.mult, op1=ALU.add),
                             reads=[K('RA'), K('IU'), ('HH', rp)], writes=[K('HH')])
                        T.op('pool', lambda e: e.tensor_tensor(out=QT[:, n, tsl], in0=HH[:, r_, :], in1=GG[:, r_, :], op=ALU.mult),
                             reads=[K('HH'), K('GG')], writes=[('ORT', n, tg)])

                    NI = len(iters)
                    for k in range(NI + 1):
                        if k < NI:
                            stage1(k)
                        if k >= 1:
                            stage2(k - 1)
                    T.barrier()
                if dbg and 'orT' in dbg:
                    with ExitStack() as dst_:
                        DT_ = sb(dst_, "DBGT2", [128, S], F32)
                        for c in range(NKC):
                            T.op('dve', lambda e, c=c: e.tensor_copy(DT_[:], QT[:, c, :]), reads=['dbgd'], writes=['dbgt'])
                            T.dma('sp', dbg_out['orT'][c * 128:(c + 1) * 128, :], DT_[:], 'dbg', reads=['dbgt'], writes=['dbgd'])
                        T.barrier()

                chk('rnn')
                branch(w_br_rnn, OFF['gr'], False)
                chk('brB')

                with ExitStack() as cst:
                    WO = sb(cst, "WO", [128, NKC, D], BF16)
                    XI = sb(cst, "XI", [128, 2, D], F32)
                    YT = sb(cst, "YT", [128, 2, D], F32)
                    LNP = sb(cst, "LNP", [128, 2, D], F32)
                    STt = sb(cst, "STt", [128, 2, 6], F32)
                    MVt = sb(cst, "MVt", [128, 2], F32)
                    RSt = sb(cst, "RSt", [128, 1], F32)
                    for hf in range(2):
                        load_w(WO[:, :, hf * 512:(hf + 1) * 512], w_o[L][:, hf * 512:(hf + 1) * 512], 'wo', 'WO')
                    T.dma('sp', LNP[:, 0, :], ln1_g[L:L + 1, :].broadcast_to([128, D]), 'lnp', writes=['LNP'])
                    T.dma('sp', LNP[:, 1, :], ln1_b[L:L + 1, :].broadcast_to([128, D]), 'lnp', writes=['LNP'])
                    T.dma('sp', XI[:, 0, :], xin_d[0:128, :], 'xi0', writes=[('XI', 0)])
                    for t in range(NB):
                        sl = t % 2
                        if t + 1 < NB:
                            T.dma('sp', XI[:, 1 - sl, :], xin_d[(t + 1) * 128:(t + 2) * 128, :], 'xi%d' % (1 - sl), writes=[('XI', 1 - sl)])
                        for hf in range(2):
                            b = next_bank()
                            for kc in range(NKC):
                                T.op('pe', lambda e, kc=kc: e.matmul(ps[:, b, :], lhsT=MT[:, kc, t * 128:(t + 1) * 128], rhs=WO[:, kc, hf * 512:(hf + 1) * 512],
                                                                     start=(kc == 0), stop=(kc == NKC - 1)), reads=['WO'], writes=[('ps', b)])
                            T.op('dve', lambda e, hf=hf, b=b: e.scalar_tensor_tensor(out=YT[:, sl, hf * 512:(hf + 1) * 512], in0=XI[:, sl, hf * 512:(hf + 1) * 512],
                                                                                    scalar=ALPHA, in1=ps[:, b, :], op0=ALU.mult, op1=ALU.add),
                                 reads=[('XI', sl), ('ps', b)], writes=[('YT', sl), ('ps', b)])
                        layer_norm_tile(YT[:, sl, :], ('YT', sl), LNP, YT[:, sl, :], ('YT', sl), (STt, MVt, RSt))
                        T.dma('sp', x1s_d[t * 128:(t + 1) * 128, :], YT[:, sl, :], 'x1o%d' % sl, reads=[('YT', sl)], writes=[('x1s', t)])
                    T.barrier()

        def ffn(seq, li, xout_d):
            L = li
            jf = li // 2
            moe = (li % 2 == 1)
            tok0 = seq * S
            build_xt(None, XT, x1s_d, NKC, 'XT')
            with ExitStack() as fst:
                ACC = sb(fst, "ACC", [128, NB, D], F32)
                CMB = sb(fst, "CMB", [128, NB, 8], F32)
                with ExitStack() as pst:
                    PTT = sb(pst, "PTT", [128, 2, S], BF16)
                    WPG = sb(pst, "WPG", [128, NKC, D], BF16)
                    WPP = sb(pst, "WPP", [128, 2, D], BF16)
                    WR = sb(pst, "WR", [128, NKC, 8], BF16)
                    SGt = sb(pst, "SGt2", [128, 2, 512], F32)
                    TMPt = sb(pst, "TMPt2", [128, 2, 512], F32)
                    LG = sb(pst, "LG", [128, 8], F32)
                    M8r = sb(pst, "M8r", [128, 8], F32)
                    GT = sb(pst, "GT", [128, 4], F32)
                    T1 = sb(pst, "T1", [128, 8], F32)
                    build_xt(None, PTT, p_in[L][tok0:tok0 + S, :], 2, 'PTT')
                    for hf in range(2):
                        load_w(WPG[:, :, hf * 512:(hf + 1) * 512], ple_w_gate[L][:, hf * 512:(hf + 1) * 512], 'wpg', 'WPG')
                        load_w(WPP[:, :, hf * 512:(hf + 1) * 512], ple_w_proj[L][:, hf * 512:(hf + 1) * 512], 'wpp', 'WPP')
                    if moe:
                        load_w(WR[:], moe_router[jf], 'wr', 'WR')
                        T.dma('sp', RB[:], moe_router_b[jf:jf + 1, :].broadcast_to([128, 8]), 'prm2', writes=['RB'])
                    k_ = [0]
                    for t in range(NB):
                        T.dma('sp', ACC[:, t, :], x1s_d[t * 128:(t + 1) * 128, :], 'acc%d' % (t % 4), writes=[('ACC', t)])
                        T.op('pool', lambda e, t=t: e.tensor_scalar(out=ACC[:, t, :], in0=ACC[:, t, :], scalar1=ALPHA, scalar2=None, op0=ALU.mult),
                             reads=[('ACC', t)], writes=[('ACC', t)])
                        for hf in range(2):
                            bG = next_bank()
                            for kc in range(NKC):
                                T.op('pe', lambda e, kc=kc: e.matmul(ps[:, bG, :], lhsT=XT[:, kc, t * 128:(t + 1) * 128], rhs=WPG[:, kc, hf * 512:(hf + 1) * 512],
                                                                     start=(kc == 0), stop=(kc == NKC - 1)), reads=['WPG'], writes=[('ps', bG)])
                            bP = next_bank()
                            for kc in range(2):
                                T.op('pe', lambda e, kc=kc: e.matmul(ps[:, bP, :], lhsT=PTT[:, kc, t * 128:(t + 1) * 128], rhs=WPP[:, kc, hf * 512:(hf + 1) * 512],
                                                                     start=(kc == 0), stop=(kc == 1)), reads=['WPP'], writes=[('ps', bP)])
                            sl = k_[0] % 2
                            k_[0] += 1
                            T.op('act', lambda e: e.activation(out=SGt[:, sl, :], in_=ps[:, bG, :], func=AF.Sigmoid), reads=[('ps', bG)], writes=[('SG', sl)])
                            T.op('dve', lambda e: e.tensor_tensor(out=TMPt[:, sl, :], in0=ps[:, bP, :], in1=SGt[:, sl, :], op=ALU.mult),
                                 reads=[('ps', bP), ('SG', sl)], writes=[('ps', bP), ('TMP', sl)])
                            T.op('pool', lambda e, t=t, hf=hf: e.tensor_tensor(out=ACC[:, t, hf * 512:(hf + 1) * 512], in0=ACC[:, t, hf * 512:(hf + 1) * 512],
                                                                               in1=TMPt[:, sl, :], op=ALU.add), reads=[('TMP', sl), ('ACC', t)], writes=[('ACC', t)])
                        if moe:
                            b = next_bank()
                            for kc in range(NKC):
                                T.op('pe', lambda e, kc=kc: e.matmul(ps[:, b, 0:8], lhsT=XT[:, kc, t * 128:(t + 1) * 128], rhs=WR[:, kc, :],
                                                                     start=(kc == 0), stop=(kc == NKC - 1)), reads=['WR'], writes=[('ps', b)])
                            T.op('dve', lambda e: e.tensor_tensor(out=LG[:], in0=ps[:, b, 0:8], in1=RB[:], op=ALU.add), reads=[('ps', b), 'RB'], writes=['LG', ('ps', b)])
                            T.op('dve', lambda e: e.max(out=M8r[:], in_=LG[:]), reads=['LG'], writes=['M8r'])
                            T.op('dve', lambda e: e.tensor_tensor(out=GT[:, 0:1], in0=M8r[:, 1:2], in1=M8r[:, 0:1], op=ALU.subtract), reads=['M8r'], writes=['GT0'])
                            T.op('act', lambda e: e.activation(out=GT[:, 1:2], in_=GT[:, 0:1], func=AF.Exp), reads=['GT0'], writes=['GT1'])
                            T.op('dve', lambda e: e.tensor_scalar(out=GT[:, 2:3], in0=GT[:, 1:2], scalar1=1.0, scalar2=None, op0=ALU.add),
                                 reads=['GT1'], writes=['GT2'])
                            T.op('dve', lambda e: e.reciprocal(GT[:, 2:3], GT[:, 2:3]), reads=['GT2'], writes=['GT2'])
                            T.op('dve', lambda e: e.tensor_tensor(out=GT[:, 3:4], in0=GT[:, 1:2], in1=GT[:, 2:3], op=ALU.mult), reads=['GT1', 'GT2'], writes=['GT3'])
                            T.op('dve', lambda e: e.tensor_scalar(out=T1[:], in0=LG[:], scalar1=M8r[:, 0:1], scalar2=GT[:, 2:3], op0=ALU.is_equal, op1=ALU.mult),
                                 reads=['LG', 'M8r', 'GT2'], writes=['T1'])
                            T.op('dve', lambda e, t=t: e.tensor_scalar(out=CMB[:, t, :], in0=LG[:], scalar1=M8r[:, 1:2], scalar2=GT[:, 3:4], op0=ALU.is_equal, op1=ALU.mult),
                                 reads=['LG', 'M8r', 'GT3'], writes=[('CMB', t)])
                            T.op('dve', lambda e, t=t: e.tensor_tensor(out=CMB[:, t, :], in0=CMB[:, t, :], in1=T1[:], op=ALU.add), reads=['T1', ('CMB', t)], writes=[('CMB', t)])
                    T.barrier()

                with ExitStack() as gst:
                    UW = 256
                    WGU = sb(gst, "WGU", [128, 2, 2, NKC, UW], BF16)
                    WDN = sb(gst, "WDN", [128, 2, UW // 128, D], BF16)
                    HT = sb(gst, "HT", [128, 2, UW // 128, S], BF16)
                    SLt = sb(gst, "SLt", [128, 2, 512], F32)
                    nexp = NEXP if moe else 1
                    dff = D_FFE if moe else D_FF
                    ui = [0]
                    k_ = [0]
                    for ex in range(nexp):
                        if moe:
                            wg_d, wu_d, wd_d = moe_w_gate[jf, ex], moe_w_up[jf, ex], moe_w_down[jf, ex]
                        else:
                            wg_d, wu_d, wd_d = ffn_w_gate[jf], ffn_w_up[jf], ffn_w_down[jf]
                        for u in range(dff // UW):
                            us = ui[0] % 2
                            ui[0] += 1
                            load_w(WGU[:, us, 0, :, :], wg_d[:, u * UW:(u + 1) * UW], 'wu%d' % us, ('WGU', us))
                            load_w(WGU[:, us, 1, :, :], wu_d[:, u * UW:(u + 1) * UW], 'wv%d' % us, ('WGU', us))
                            load_w(WDN[:, us, :, :], wd_d[u * UW:(u + 1) * UW, :], 'wd%d' % us, ('WDN', us))
                            for cl in range(UW // 128):
                                for tg in range(4):
                                    bg = next_bank()
                                    for kc in range(NKC):
                                        T.op('pe', lambda e, kc=kc: e.matmul(ps[:, bg, :], lhsT=WGU[:, us, 0, kc, cl * 128:(cl + 1) * 128],
                                                                             rhs=XT[:, kc, tg * 512:(tg + 1) * 512], start=(kc == 0), stop=(kc == NKC - 1)),
                                             reads=[('WGU', us)], writes=[('ps', bg)])
                                    bu = next_bank()
                                    for kc in range(NKC):
                                        T.op('pe', lambda e, kc=kc: e.matmul(ps[:, bu, :], lhsT=WGU[:, us, 1, kc, cl * 128:(cl + 1) * 128],
                                                                             rhs=XT[:, kc, tg * 512:(tg + 1) * 512], start=(kc == 0), stop=(kc == NKC - 1)),
                                             reads=[('WGU', us)], writes=[('ps', bu)])
                                    sl = k_[0] % 2
                                    k_[0] += 1
                                    T.op('act', lambda e: e.activation(out=SLt[:, sl, :], in_=ps[:, bg, :], func=AF.Silu), reads=[('ps', bg)], writes=[('SL', sl)])
                                    T.op('dve', lambda e: e.tensor_tensor(out=HT[:, us, cl, tg * 512:(tg + 1) * 512], in0=ps[:, bu, :], in1=SLt[:, sl, :], op=ALU.mult),
                                         reads=[('ps', bu), ('SL', sl)], writes=[('ps', bu), ('HT', us)])
                            for t in range(NB):
                                for hf in range(2):
                                    b = next_bank()
                                    ncl = UW // 128
                                    for cl in range(ncl):
                                        T.op('pe', lambda e, cl=cl: e.matmul(ps[:, b, :], lhsT=HT[:, us, cl, t * 128:(t + 1) * 128],
                                                                             rhs=WDN[:, us, cl, hf * 512:(hf + 1) * 512], start=(cl == 0), stop=(cl == ncl - 1)),
                                             reads=[('HT', us), ('WDN', us)], writes=[('ps', b)])
                                    if moe:
                                        T.op('dve', lambda e, t=t, hf=hf, b=b, ex=ex: e.scalar_tensor_tensor(
                                            out=ACC[:, t, hf * 512:(hf + 1) * 512], in0=ps[:, b, :], scalar=CMB[:, t, ex:ex + 1],
                                            in1=ACC[:, t, hf * 512:(hf + 1) * 512], op0=ALU.mult, op1=ALU.add),
                                             reads=[('ps', b), ('ACC', t, hf), ('CMB', t)], writes=[('ps', b), ('ACC', t, hf)])
                                    else:
                                        T.op('dve', lambda e, t=t, hf=hf, b=b: e.tensor_tensor(
                                            out=ACC[:, t, hf * 512:(hf + 1) * 512], in0=ps[:, b, :], in1=ACC[:, t, hf * 512:(hf + 1) * 512], op=ALU.add),
                                             reads=[('ps', b), ('ACC', t, hf)], writes=[('ps', b), ('ACC', t, hf)])
                    T.barrier()

                with ExitStack() as lst:
                    LNP = sb(lst, "LNP2", [128, 2, D], F32)
                    STt = sb(lst, "STt2", [128, 2, 6], F32)
                    MVt = sb(lst, "MVt2", [128, 2], F32)
                    RSt = sb(lst, "RSt2", [128, 1], F32)
                    T.dma('sp', LNP[:, 0, :], ln2_g[L:L + 1, :].broadcast_to([128, D]), 'lnp', writes=['LNP'])
                    T.dma('sp', LNP[:, 1, :], ln2_b[L:L + 1, :].broadcast_to([128, D]), 'lnp', writes=['LNP'])
                    for t in range(NB):
                        layer_norm_tile(ACC[:, t, :], ('ACC', t), LNP, ACC[:, t, :], ('ACC', t), (STt, MVt, RSt))
                        T.dma('sp', xout_d[t * 128:(t + 1) * 128, :], ACC[:, t, :], 'xo%d' % (t % 4), reads=[('ACC', t)], writes=[('xo', t)])
                    T.barrier()

        def chk(name):
            if stop == name and not T.dead:
                T.barrier()
                T.final_wait('sp')
                T.dead = True

        try:
            chk('prologue')
            for seq in range(nseq):
                tok0 = seq * S
                for idx, li in enumerate(layers):
                    xin_d = x_in[tok0:tok0 + S, :] if idx == 0 else xs_d
                    xout_d = y_out[tok0:tok0 + S, :] if idx == len(layers) - 1 else xs_d
                    mixer(seq, li, xin_d)
                    chk('mixer')
                    ffn(seq, li, xout_d)
        except StopBuild:
            T.barrier()
        T.final_wait('sp')
        build.n_ins = T.n_ins
    return nc


_CACHE = {}


def host_inputs(inputs, nseq_total, ncores):
    consts = make_consts()
    rb = np.asarray(inputs['rel_bias'], dtype=np.float32)
    tabs = bias_index_tables()
    heads = np.array([head_of(g, i) for g in range(4) for i in range(4)])
    biasg = np.stack([rb[tabs[k]][:, :, heads].transpose(0, 2, 1).reshape(128, 16 * 128) for k in range(2)]).astype(np.float32)
    biasc = np.broadcast_to(rb[15, heads][None, :, None], (128, 16, 128)).reshape(128, 16 * 128).astype(np.float32)
    shared = {k: np.ascontiguousarray(np.asarray(v, dtype=np.float32)) for k, v in inputs.items() if k not in ('x', 'p', 'rel_bias')}
    shared.update(c_ident=consts['ident'], c_i4=consts['i4'], c_admneg=consts['admneg'], c_adm01=consts['adm01'],
                  c_biasg=np.ascontiguousarray(biasg), c_biasc=np.ascontiguousarray(biasc))
    x = np.asarray(inputs['x'], dtype=np.float32)
    p = np.asarray(inputs['p'], dtype=np.float32)
    per = nseq_total // ncores
    in_maps = []
    for c in range(ncores):
        m = dict(shared)
        m['x'] = np.ascontiguousarray(x[c * per:(c + 1) * per].reshape(per * S, D))
        m['p'] = np.ascontiguousarray(p[:, c * per:(c + 1) * per].reshape(DEPTH, per * S, PLE))
        in_maps.append(m)
    return in_maps


def kernel(**inputs):
    B = inputs['x'].shape[0]
    per = B // N_CORES
    key = ('full', per)
    if key not in _CACHE:
        _CACHE[key] = build(per, list(range(DEPTH)))
    nc = _CACHE[key]
    in_maps = host_inputs(inputs, B, N_CORES)
    res = run_bass_kernel_spmd(nc, in_maps, core_ids=list(range(N_CORES)))
    out = np.concatenate([np.asarray(r['y']).reshape(per, S, D) for r in res.results], axis=0)
    return out.astype(np.float32)
```
